# Optimizing a Trainium2 kernel written in Bass

```python
import math
import jax, jax.numpy as jnp
from jax import lax
import numpy as np

D_MODEL = 2048
BATCH = 2
SEQ = 16384
DEPTH = 2

GRID_W = 64
CTX_LEN = 256
N_BRANCH = 4
MIX_W = D_MODEL // N_BRANCH
HEAD_DIM = 128
EPS = 1e-6
ROPE_BASE = 10000.0
HY_W = MIX_W
HY_ORDER = 2
HY_CONV = 3
HY_BANDS = 16
HY_EMB_DIM = 2 * HY_BANDS + 1
HY_FFN = 64
HY_DECAY_TARGET = 1e-2
HY_FAST_PCT = 0.3
HY_SLOW_PCT = 1.5
NA_HEADS = MIX_W // HEAD_DIM
NA_WIN_R = 8
NA_WIN_C = 16
RET_HEADS = MIX_W // HEAD_DIM
RET_DK = HEAD_DIM // 2
RET_DV = HEAD_DIM
RET_CHUNK = 128
RET_ROPE_BASE = 10000.0
GQA_Q_HEADS = MIX_W // HEAD_DIM
GQA_KV_HEADS = max(GQA_Q_HEADS // 2, 1)
GQA_BLOCK = 128
FFN_HIDDEN = ((8 * D_MODEL + 3 * 256 - 1) // (3 * 256)) * 256
SPLIT_SIZES = (3 * HY_W,
               NA_HEADS * HEAD_DIM, NA_HEADS * HEAD_DIM, NA_HEADS * HEAD_DIM,
               RET_HEADS * RET_DK, RET_HEADS * RET_DK, RET_HEADS * RET_DV, RET_HEADS * RET_DV,
               GQA_Q_HEADS * HEAD_DIM, GQA_KV_HEADS * HEAD_DIM, GQA_KV_HEADS * HEAD_DIM,
               N_BRANCH * D_MODEL)
IN_COLS = (3 * HY_W + 3 * NA_HEADS * HEAD_DIM + 2 * RET_HEADS * RET_DK + 2 * RET_HEADS * RET_DV
           + (GQA_Q_HEADS + 2 * GQA_KV_HEADS) * HEAD_DIM + N_BRANCH * D_MODEL)

kernel_name = 'hybrid_flow_backbone'


def rms_norm(x, w):
    xf = x.astype(jnp.float32)
    y = xf * lax.rsqrt(jnp.mean(xf * xf, axis=-1, keepdims=True) + EPS)
    return (y * w.astype(jnp.float32)).astype(x.dtype)


def modulate(xn, shift, scale):
    return xn * (1 + scale) + shift


def rotate_pairs(x, ang):
    m = x.shape[-1] // 2
    x1, x2 = x[..., :m], x[..., m:]
    cos, sin = jnp.cos(ang), jnp.sin(ang)
    return jnp.concatenate([x1 * cos - x2 * sin, x1 * sin + x2 * cos], axis=-1)


def axial_rope(x):
    n, d = x.shape[1], x.shape[-1]
    nf = d // 4
    t = jnp.arange(n)
    inv = ROPE_BASE ** (-jnp.arange(nf, dtype=jnp.float32) / nf)
    ang_r = (t // GRID_W).astype(jnp.float32)[:, None] * inv
    ang_c = (t % GRID_W).astype(jnp.float32)[:, None] * inv
    xf = x.astype(jnp.float32)
    out = jnp.concatenate([rotate_pairs(xf[..., :d // 2], ang_r[:, None, :]),
                           rotate_pairs(xf[..., d // 2:], ang_c[:, None, :])], axis=-1)
    return out.astype(x.dtype)


def retnet_rope(x):
    n, d = x.shape[1], x.shape[-1]
    inv = RET_ROPE_BASE ** (-jnp.linspace(0.0, 1.0, d // 2, dtype=jnp.float32))
    ang = jnp.arange(n, dtype=jnp.float32)[:, None] * inv
    return rotate_pairs(x.astype(jnp.float32), ang[:, None, :])


def split_proj(p):
    idx = np.cumsum(np.array(SPLIT_SIZES))[:-1].tolist()
    return jnp.split(p, idx, axis=-1)


def ctx_attention(q, k, v):
    B, L, Hq, d = q.shape
    Hkv = k.shape[2]
    qg = q.reshape(B, L, Hkv, Hq // Hkv, d)
    s = jnp.einsum('bqkgd,bskd->bkgqs', qg, k).astype(jnp.float32) * d ** -0.5
    p = jax.nn.softmax(s, axis=-1).astype(v.dtype)
    return jnp.einsum('bkgqs,bskd->bqkgd', p, v).reshape(B, L, Hq * d)


def hyena_filter_spectra(L, w1, b1, w2, b2, w3, sin_freq):
    t = jnp.arange(L, dtype=jnp.float32)
    t_norm = t / max(L - 1, 1)
    w = 2.0 * math.pi * t / L
    f = jnp.linspace(1e-4, HY_BANDS - 1, HY_BANDS, dtype=jnp.float32)
    feat = jnp.concatenate([t_norm[:, None], jnp.cos(w[:, None] * f), -jnp.sin(w[:, None] * f)], axis=-1)
    z = jnp.sin(sin_freq[0] * (feat @ w1 + b1))
    z = jnp.sin(sin_freq[1] * (z @ w2 + b2))
    h = (z @ w3).astype(jnp.float32).reshape(L, HY_ORDER, 2, HY_W)
    deltas = jnp.abs(jnp.linspace(math.log(HY_DECAY_TARGET) / HY_SLOW_PCT,
                                  math.log(HY_DECAY_TARGET) / HY_FAST_PCT, HY_W, dtype=jnp.float32))
    h = h * jnp.exp(-t_norm[:, None] * deltas)[:, None, None, :]
    h_fwd, h_bwd = h[:, :, 0], h[:, :, 1]
    filt = jnp.concatenate([h_fwd, jnp.zeros((1, HY_ORDER, HY_W), jnp.float32), h_bwd[:0:-1]], axis=0)
    filt = filt * lax.rsqrt(jnp.sum(filt * filt, axis=0, keepdims=True) + EPS)
    return jnp.fft.rfft(filt, axis=0)


def long_conv(z, spec):
    L = z.shape[1]
    zf = jnp.fft.rfft(z, n=2 * L, axis=1)
    return jnp.fft.irfft(zf * spec[None], n=2 * L, axis=1)[:, :L]


def short_conv(u, w, b):
    C = u.shape[-1]
    y = lax.conv_general_dilated(u, w[:, None, :].astype(u.dtype), window_strides=(1,),
                                 padding=((HY_CONV // 2, HY_CONV // 2),),
                                 dimension_numbers=('NWC', 'WIO', 'NWC'), feature_group_count=C)
    return y + b


def hyena_mixer(p, conv_w, conv_b, w1, b1, w2, b2, w3, sin_freq, bias):
    L = p.shape[1]
    u = short_conv(p, conv_w, conv_b).astype(jnp.float32)
    v, x1, x2 = jnp.split(u, 3, axis=-1)
    spec = hyena_filter_spectra(L, w1, b1, w2, b2, w3, sin_freq)
    bias = bias.astype(jnp.float32)
    z = v
    for o, gate in enumerate((x1, x2)):
        z = gate * (long_conv(z, spec[:, o]) + bias[o] * z)
    return z.astype(p.dtype)


def na_mixer(q_l, k_l, v_l, q_c, k_c, v_c, rpb, with_ctx_out):
    B, N = q_l.shape[:2]
    Lc = q_c.shape[1]
    R = N // GRID_W
    WR = min(NA_WIN_R, R)
    H, d = NA_HEADS, HEAD_DIM
    scale = d ** -0.5
    qg = q_l.reshape(B, R, GRID_W, H, d)
    kg = k_l.reshape(B, R, GRID_W, H, d)
    vg = v_l.reshape(B, R, GRID_W, H, d)
    kc = k_c.reshape(B, Lc, H, d)
    vc = v_c.reshape(B, Lc, H, d)
    cols = jnp.arange(GRID_W)
    c0 = jnp.clip(cols - NA_WIN_C // 2, 0, GRID_W - NA_WIN_C)
    col_ok = (cols[None, :] >= c0[:, None]) & (cols[None, :] < c0[:, None] + NA_WIN_C)
    col_idx = jnp.clip(cols[None, :] - cols[:, None] + NA_WIN_C - 1, 0, 2 * NA_WIN_C - 2)
    rpb_cols = rpb[:, :, col_idx].astype(jnp.float32)

    def row(args):
        q_r, r = args
        r0 = jnp.clip(r - WR // 2, 0, R - WR)
        k_rows = lax.dynamic_slice_in_dim(kg, r0, WR, axis=1)
        v_rows = lax.dynamic_slice_in_dim(vg, r0, WR, axis=1)
        s_lat = jnp.einsum('bqhd,bwkhd->bhqwk', q_r, k_rows).astype(jnp.float32) * scale
        bias = jnp.take(rpb_cols, r0 + jnp.arange(WR) - r + NA_WIN_R - 1, axis=1)
        s_lat = jnp.where(col_ok[:, None, :], s_lat + jnp.transpose(bias, (0, 2, 1, 3)), -jnp.inf)
        s_ctx = jnp.einsum('bqhd,bchd->bhqc', q_r, kc).astype(jnp.float32) * scale
        s = jnp.concatenate([s_lat.reshape(B, H, GRID_W, WR * GRID_W), s_ctx], axis=-1)
        p = jax.nn.softmax(s, axis=-1).astype(v_l.dtype)
        p_lat = p[..., :WR * GRID_W].reshape(B, H, GRID_W, WR, GRID_W)
        return (jnp.einsum('bhqwk,bwkhd->bqhd', p_lat, v_rows)
                + jnp.einsum('bhqc,bchd->bqhd', p[..., WR * GRID_W:], vc))

    y = lax.map(row, (jnp.moveaxis(qg, 1, 0), jnp.arange(R)))
    y_l = jnp.moveaxis(y, 0, 1).reshape(B, N, H * d)
    y_c = ctx_attention(q_c.reshape(B, Lc, H, d), kc, vc) if with_ctx_out else None
    return y_l, y_c


def retention_chunkwise(q, k, v, log_g, s0):
    B, H, L, dk = q.shape
    dv = v.shape[-1]
    C = RET_CHUNK
    n = L // C
    qc, kc, vc = q.reshape(B, H, n, C, dk), k.reshape(B, H, n, C, dk), v.reshape(B, H, n, C, dv)
    j = jnp.arange(C, dtype=jnp.float32)
    rel = j[:, None] - j[None, :]
    intra = jnp.where(rel >= 0, jnp.exp(log_g[:, None, None] * jnp.maximum(rel, 0.0)), 0.0)
    scores = jnp.einsum('bhncd,bhnmd->bhncm', qc, kc) * intra[None, :, None]
    inner = jnp.einsum('bhncm,bhnme->bhnce', scores, vc)
    k_dec = kc * jnp.exp(log_g[:, None] * (C - 1 - j))[None, :, None, :, None]
    kv = jnp.einsum('bhncd,bhnce->bhnde', k_dec, vc)
    chunk_decay = jnp.exp(log_g * C)[None, :, None, None]

    def step(state, kv_i):
        return chunk_decay * state + kv_i, state

    s_final, s_prev = lax.scan(step, s0, jnp.moveaxis(kv, 2, 0))
    cross = (jnp.einsum('bhncd,nbhde->bhnce', qc, s_prev)
             * jnp.exp(log_g[:, None] * (j + 1))[None, :, None, :, None])
    return (inner + cross).reshape(B, H, L, dv), s_final


def retention_out(o, g, gn_w):
    o = o * lax.rsqrt(jnp.mean(o * o, axis=-1, keepdims=True) + EPS)
    B, H, L, dv = o.shape
    o = jnp.moveaxis(o, 1, 2).reshape(B, L, H * dv) * gn_w.astype(jnp.float32)
    return (o * jax.nn.silu(g.astype(jnp.float32))).astype(g.dtype)


def retention_mixer(q_l, k_l, v_l, g_l, q_c, k_c, v_c, g_c, log_decay, gn_w, with_ctx_out):
    def heads(t, d):
        return t.reshape(t.shape[0], t.shape[1], RET_HEADS, d)

    def bhld(t):
        return jnp.moveaxis(t, 2, 1).astype(jnp.float32)

    ksc = RET_DK ** -0.5
    ql = bhld(retnet_rope(heads(q_l, RET_DK)))
    kl = bhld(retnet_rope(heads(k_l, RET_DK))) * ksc
    vl = bhld(heads(v_l, RET_DV))
    qc = bhld(heads(q_c, RET_DK))
    kc = bhld(heads(k_c, RET_DK)) * ksc
    vc = bhld(heads(v_c, RET_DV))
    log_g = -jnp.abs(log_decay.astype(jnp.float32))
    s0 = jnp.zeros((q_l.shape[0], RET_HEADS, RET_DK, RET_DV), jnp.float32)

    def flip(t):
        return jnp.flip(t, axis=2)

    o_cf, s_fwd = retention_chunkwise(qc, kc, vc, log_g[0], s0)
    o_cb, s_bwd = retention_chunkwise(flip(qc), flip(kc), flip(vc), log_g[1], s0)
    o_lf, _ = retention_chunkwise(ql, kl, vl, log_g[0], s_fwd)
    o_lb, _ = retention_chunkwise(flip(ql), flip(kl), flip(vl), log_g[1], s_bwd)
    y_l = retention_out(o_lf + flip(o_lb), g_l, gn_w)
    y_c = retention_out(o_cf + flip(o_cb), g_c, gn_w) if with_ctx_out else None
    return y_l, y_c


def gqa_mixer(q_l, k_l, v_l, q_c, k_c, v_c, qn_w, kn_w, with_ctx_out):
    B, N = q_l.shape[:2]
    Lc = q_c.shape[1]
    G = GQA_Q_HEADS // GQA_KV_HEADS
    ql = axial_rope(rms_norm(q_l.reshape(B, N, GQA_Q_HEADS, HEAD_DIM), qn_w))
    kl = axial_rope(rms_norm(k_l.reshape(B, N, GQA_KV_HEADS, HEAD_DIM), kn_w))
    vl = v_l.reshape(B, N, GQA_KV_HEADS, HEAD_DIM)
    qc = rms_norm(q_c.reshape(B, Lc, GQA_Q_HEADS, HEAD_DIM), qn_w)
    kc = rms_norm(k_c.reshape(B, Lc, GQA_KV_HEADS, HEAD_DIM), kn_w)
    vc = v_c.reshape(B, Lc, GQA_KV_HEADS, HEAD_DIM)
    keys = jnp.concatenate([kl, kc], axis=1)
    vals = jnp.concatenate([vl, vc], axis=1)
    nb = N // GQA_BLOCK
    qb = jnp.moveaxis(ql.reshape(B, nb, GQA_BLOCK, GQA_KV_HEADS, G, HEAD_DIM), 1, 0)

    def block(qi):
        s = jnp.einsum('bqkgd,bskd->bkgqs', qi, keys).astype(jnp.float32) * HEAD_DIM ** -0.5
        p = jax.nn.softmax(s, axis=-1).astype(vals.dtype)
        return jnp.einsum('bkgqs,bskd->bqkgd', p, vals)

    y_l = jnp.moveaxis(lax.map(block, qb), 0, 1).reshape(B, N, GQA_Q_HEADS * HEAD_DIM)
    y_c = ctx_attention(qc, kc, vc) if with_ctx_out else None
    return y_l, y_c


def merge_branches(branches, gate_pre, w_branch, w_out):
    B, L = gate_pre.shape[:2]
    gates = jax.nn.sigmoid(gate_pre.astype(jnp.float32)).astype(gate_pre.dtype)
    gates = gates.reshape(B, L, N_BRANCH, D_MODEL)
    m = gates[:, :, 0] * (branches[0] @ w_branch[0])
    for i in range(1, N_BRANCH):
        m = m + gates[:, :, i] * (branches[i] @ w_branch[i])
    return m @ w_out


def token_mixing(h_l, h_c, w_in, hy_conv_w, hy_conv_b, hy_ffn_w1, hy_ffn_b1, hy_ffn_w2, hy_ffn_b2,
                 hy_ffn_w3, hy_sin_freq, hy_bias, na_rpb, ret_log_decay, ret_gn_w,
                 gqa_q_norm_w, gqa_k_norm_w, w_branch, w_out, with_ctx_out):
    (hy_l, naq_l, nak_l, nav_l, rq_l, rk_l, rv_l, rg_l, gq_l, gk_l, gv_l, gate_l) = split_proj(h_l @ w_in)
    (hy_c, naq_c, nak_c, nav_c, rq_c, rk_c, rv_c, rg_c, gq_c, gk_c, gv_c, gate_c) = split_proj(h_c @ w_in)
    hy_params = (hy_conv_w, hy_conv_b, hy_ffn_w1, hy_ffn_b1, hy_ffn_w2, hy_ffn_b2, hy_ffn_w3, hy_sin_freq, hy_bias)
    y_hy_l = hyena_mixer(hy_l, *hy_params)
    y_na_l, y_na_c = na_mixer(naq_l, nak_l, nav_l, naq_c, nak_c, nav_c, na_rpb, with_ctx_out)
    y_rt_l, y_rt_c = retention_mixer(rq_l, rk_l, rv_l, rg_l, rq_c, rk_c, rv_c, rg_c,
                                     ret_log_decay, ret_gn_w, with_ctx_out)
    y_gq_l, y_gq_c = gqa_mixer(gq_l, gk_l, gv_l, gq_c, gk_c, gv_c, gqa_q_norm_w, gqa_k_norm_w, with_ctx_out)
    out_l = merge_branches((y_hy_l, y_na_l, y_rt_l, y_gq_l), gate_l, w_branch, w_out)
    if not with_ctx_out:
        return out_l, None
    y_hy_c = hyena_mixer(hy_c, *hy_params)
    out_c = merge_branches((y_hy_c, y_na_c, y_rt_c, y_gq_c), gate_c, w_branch, w_out)
    return out_l, out_c


def swiglu(h, w13, w2):
    a, b = jnp.split(h @ w13, 2, axis=-1)
    return (jax.nn.silu(a) * b) @ w2


def setup_inputs(seed: int = 0) -> dict:
    key = jax.random.key(seed)
    ks = jax.random.split(key, 32)
    nrm = jax.random.normal
    D = D_MODEL

    def w(k, shape, fan_in, gain=1.0):
        return nrm(k, shape, jnp.float32) * (gain * fan_in ** -0.5)

    ret_init = jnp.log(1.0 - 2.0 ** (-5.0 - jnp.arange(RET_HEADS, dtype=jnp.float32)))
    return {
        'x': nrm(ks[0], (BATCH, SEQ, D), jnp.float32),
        'c': nrm(ks[1], (BATCH, D), jnp.float32),
        'ctx': nrm(ks[2], (BATCH, CTX_LEN, D), jnp.float32),
        'c_ctx': nrm(ks[3], (D,), jnp.float32),
        'ada_w': w(ks[4], (DEPTH, D, 6 * D), D, 0.5),
        'ada_b': 0.01 * nrm(ks[5], (DEPTH, 6 * D), jnp.float32),
        'norm1_w': 1.0 + 0.02 * nrm(ks[6], (DEPTH, D), jnp.float32),
        'norm2_w': 1.0 + 0.02 * nrm(ks[7], (DEPTH, D), jnp.float32),
        'w_in': w(ks[8], (DEPTH, D, IN_COLS), D),
        'hy_conv_w': w(ks[9], (DEPTH, HY_CONV, 3 * HY_W), HY_CONV),
        'hy_conv_b': 0.01 * nrm(ks[10], (DEPTH, 3 * HY_W), jnp.float32),
        'hy_ffn_w1': w(ks[11], (DEPTH, HY_EMB_DIM, HY_FFN), HY_EMB_DIM),
        'hy_ffn_b1': 0.1 * nrm(ks[12], (DEPTH, HY_FFN), jnp.float32),
        'hy_ffn_w2': w(ks[13], (DEPTH, HY_FFN, HY_FFN), HY_FFN),
        'hy_ffn_b2': 0.1 * nrm(ks[14], (DEPTH, HY_FFN), jnp.float32),
        'hy_ffn_w3': w(ks[15], (DEPTH, HY_FFN, HY_ORDER * 2 * HY_W), HY_FFN),
        'hy_sin_freq': 1.0 + 0.02 * nrm(ks[16], (DEPTH, 2, HY_FFN), jnp.float32),
        'hy_bias': nrm(ks[17], (DEPTH, HY_ORDER, HY_W), jnp.float32),
        'na_rpb': 0.02 * nrm(ks[18], (DEPTH, NA_HEADS, 2 * NA_WIN_R - 1, 2 * NA_WIN_C - 1), jnp.float32),
        'ret_log_decay': ret_init * (1.0 + 0.02 * nrm(ks[19], (DEPTH, 2, RET_HEADS), jnp.float32)),
        'ret_gn_w': 1.0 + 0.02 * nrm(ks[20], (DEPTH, RET_HEADS * RET_DV), jnp.float32),
        'gqa_q_norm_w': 1.0 + 0.02 * nrm(ks[21], (DEPTH, HEAD_DIM), jnp.float32),
        'gqa_k_norm_w': 1.0 + 0.02 * nrm(ks[22], (DEPTH, HEAD_DIM), jnp.float32),
        'w_branch': w(ks[23], (DEPTH, N_BRANCH, MIX_W, D), MIX_W),
        'w_out': w(ks[24], (DEPTH, D, D), D),
        'ffn_w13': w(ks[25], (DEPTH, D, 2 * FFN_HIDDEN), D),
        'ffn_w2': w(ks[26], (DEPTH, FFN_HIDDEN, D), FFN_HIDDEN),
        'final_norm_w': 1.0 + 0.02 * nrm(ks[27], (D,), jnp.float32),
    }


def reference(x, c, ctx, c_ctx, ada_w, ada_b, norm1_w, norm2_w, w_in, hy_conv_w, hy_conv_b,
              hy_ffn_w1, hy_ffn_b1, hy_ffn_w2, hy_ffn_b2, hy_ffn_w3, hy_sin_freq, hy_bias, na_rpb,
              ret_log_decay, ret_gn_w, gqa_q_norm_w, gqa_k_norm_w, w_branch, w_out, ffn_w13, ffn_w2,
              final_norm_w):
    x_l, x_c = x, ctx
    for layer in range(DEPTH):
        last = layer == DEPTH - 1
        mod_l = (jax.nn.silu(c) @ ada_w[layer] + ada_b[layer])[:, None, :]
        mod_c = jax.nn.silu(c_ctx) @ ada_w[layer] + ada_b[layer]
        sh1_l, sc1_l, g1_l, sh2_l, sc2_l, g2_l = jnp.split(mod_l, 6, axis=-1)
        sh1_c, sc1_c, g1_c, sh2_c, sc2_c, g2_c = jnp.split(mod_c, 6, axis=-1)
        h_l = modulate(rms_norm(x_l, norm1_w[layer]), sh1_l, sc1_l)
        h_c = modulate(rms_norm(x_c, norm1_w[layer]), sh1_c, sc1_c)
        m_l, m_c = token_mixing(h_l, h_c, w_in[layer], hy_conv_w[layer], hy_conv_b[layer],
                                hy_ffn_w1[layer], hy_ffn_b1[layer], hy_ffn_w2[layer], hy_ffn_b2[layer],
                                hy_ffn_w3[layer], hy_sin_freq[layer], hy_bias[layer], na_rpb[layer],
                                ret_log_decay[layer], ret_gn_w[layer], gqa_q_norm_w[layer],
                                gqa_k_norm_w[layer], w_branch[layer], w_out[layer], not last)
        x_l = x_l + g1_l * m_l
        h_l = modulate(rms_norm(x_l, norm2_w[layer]), sh2_l, sc2_l)
        x_l = x_l + g2_l * swiglu(h_l, ffn_w13[layer], ffn_w2[layer])
        if not last:
            x_c = x_c + g1_c * m_c
            h_c = modulate(rms_norm(x_c, norm2_w[layer]), sh2_c, sc2_c)
            x_c = x_c + g2_c * swiglu(h_c, ffn_w13[layer], ffn_w2[layer])
    return rms_norm(x_l, final_norm_w)
```

```python
import math
import contextlib
import numpy as np
import concourse.bass as bass
import concourse.mybir as mybir
from concourse.bass_utils import run_bass_kernel_spmd

F32 = mybir.dt.float32
BF16 = mybir.dt.bfloat16
ALU = mybir.AluOpType
AF = mybir.ActivationFunctionType
AX = mybir.AxisListType


class Buf:
    def __init__(self, t, name, multi=False):
        self.t = t
        self.name = name
        self.multi = multi
        self.w = {}
        self.r = {}

    def __getitem__(self, idx):
        return View(self, self.t[idx])

    def v(self, ap):
        return View(self, ap)


class View:
    def __init__(self, buf, ap):
        self.buf = buf
        self.ap = ap

    def __getitem__(self, idx):
        return View(self.buf, self.ap[idx])

    def re(self, pat, **kw):
        return View(self.buf, self.ap.rearrange(pat, **kw))


class Op:
    __slots__ = ("eng", "fn", "deps", "needed", "val", "slot", "is_dma", "idx", "key")

    def __init__(self, eng, fn, is_dma):
        self.eng = eng
        self.fn = fn
        self.deps = []
        self.needed = False
        self.val = None
        self.slot = None
        self.is_dma = is_dma


def _ap(x):
    return x.ap if isinstance(x, View) else x


class Prog:
    NSLOT = 6

    def __init__(self, nc, ctx):
        self.nc = nc
        self.ctx = ctx
        self.top = ctx
        self.E = {"pe": nc.tensor, "act": nc.scalar, "dve": nc.vector, "pool": nc.gpsimd, "sp": nc.sync}
        self.ops = {k: [] for k in self.E}
        self.nbuf = 0
        self.ndma = {}

    def sb(self, shape, dt, name=None, multi=False):
        self.nbuf += 1
        name = f"s{self.nbuf}_" + (name or "")
        t = self.ctx.enter_context(self.nc.sbuf_tensor(name, list(shape), dt))
        return Buf(t, name, multi)

    def ps(self, shape, dt, name=None):
        self.nbuf += 1
        name = f"p{self.nbuf}_" + (name or "")
        t = self.ctx.enter_context(self.nc.psum_tensor(name, list(shape), dt))
        return Buf(t, name)

    def dram(self, name, shape, dt, kind="Internal"):
        t = self.nc.dram_tensor(name, list(shape), dt, kind=kind)
        return Buf(t.ap(), name, multi=True)

    def op(self, eng, fn, reads=(), writes=(), is_dma=False):
        o = Op(eng, fn, is_dma)
        if is_dma:
            n = self.ndma.get(eng, 0)
            self.ndma[eng] = n + 1
            o.slot = (eng, n % self.NSLOT)
            o.key = o.slot
        else:
            o.key = eng
        deps = []
        for b in reads:
            deps.extend(b.w.values())
        for b in writes:
            if not b.multi:
                deps.extend(b.w.values())
            deps.extend(b.r.values())
        seen = set()
        for d in deps:
            if id(d) in seen or d is o:
                continue
            seen.add(id(d))
            if d.eng == eng and eng == "pe" and not d.is_dma and not is_dma:
                continue
            o.deps.append(d)
            d.needed = True
        for b in reads:
            b.r[o.key] = o
        for b in writes:
            if b.multi:
                b.w[o.key] = o
            else:
                b.w = {o.key: o}
            b.r = {}
        self.ops[eng].append(o)
        return o

    @staticmethod
    def _bufs(*xs):
        return [x.buf for x in xs if isinstance(x, View)]

    def dma(self, out, in_, eng="sp", **kw):
        o_, i_ = _ap(out), _ap(in_)
        return self.op(eng, lambda e: e.dma_start(out=o_, in_=i_, **kw), self._bufs(in_), self._bufs(out), is_dma=True)

    def mm(self, out, lhsT, rhs, start=True, stop=True, **kw):
        o_, l_, r_ = _ap(out), _ap(lhsT), _ap(rhs)
        return self.op("pe", lambda e: e.matmul(o_, l_, r_, start=start, stop=stop, **kw), self._bufs(lhsT, rhs), self._bufs(out))

    def tr(self, out, in_, ident):
        o_, i_, d_ = _ap(out), _ap(in_), _ap(ident)
        return self.op("pe", lambda e: e.transpose(o_, i_, d_), self._bufs(in_, ident), self._bufs(out))

    def act(self, out, in_, func, bias=None, scale=None, accum_out=None):
        o_, i_ = _ap(out), _ap(in_)
        kw = {}
        rd = self._bufs(in_)
        wr = self._bufs(out)
        if bias is not None:
            kw["bias"] = _ap(bias)
            rd += self._bufs(bias)
        if scale is not None:
            kw["scale"] = _ap(scale)
            rd += self._bufs(scale)
        if accum_out is not None:
            kw["accum_out"] = _ap(accum_out)
            wr += self._bufs(accum_out)
        return self.op("act", lambda e: e.activation(o_, i_, func, **kw), rd, wr)

    def ts(self, eng, out, in0, s1, s2, op0, op1=None, accum_out=None):
        o_, i_ = _ap(out), _ap(in0)
        a1, a2 = _ap(s1), _ap(s2)
        kw = {}
        wr = self._bufs(out)
        if op1 is not None:
            kw["op1"] = op1
        if accum_out is not None:
            kw["accum_out"] = _ap(accum_out)
            wr += self._bufs(accum_out)
        return self.op(eng, lambda e: e.tensor_scalar(o_, i_, a1, a2, op0, **kw), self._bufs(in0, s1, s2), wr)

    def tt(self, eng, out, in0, in1, op):
        o_, a_, b_ = _ap(out), _ap(in0), _ap(in1)
        return self.op(eng, lambda e: e.tensor_tensor(o_, a_, b_, op), self._bufs(in0, in1), self._bufs(out))

    def stt(self, out, in0, scalar, in1, op0, op1, accum_out=None, eng="dve"):
        o_, a_, s_, b_ = _ap(out), _ap(in0), _ap(scalar), _ap(in1)
        kw = {}
        wr = self._bufs(out)
        if accum_out is not None:
            kw["accum_out"] = _ap(accum_out)
            wr += self._bufs(accum_out)
        return self.op(eng, lambda e: e.scalar_tensor_tensor(o_, a_, s_, b_, op0, op1, **kw), self._bufs(in0, scalar, in1), wr)

    def copy(self, eng, out, in_):
        o_, i_ = _ap(out), _ap(in_)
        if eng == "act":
            return self.op(eng, lambda e: e.copy(o_, i_), self._bufs(in_), self._bufs(out))
        return self.op(eng, lambda e: e.tensor_copy(o_, i_), self._bufs(in_), self._bufs(out))

    def memset(self, eng, out, val):
        o_ = _ap(out)
        return self.op(eng, lambda e: e.memset(o_, val), [], self._bufs(out))

    def reduce(self, out, in_, op, axis=AX.X, eng="dve"):
        o_, i_ = _ap(out), _ap(in_)
        return self.op(eng, lambda e: e.tensor_reduce(o_, i_, axis, op), self._bufs(in_), self._bufs(out))

    def recip(self, out, in_, eng="dve"):
        o_, i_ = _ap(out), _ap(in_)
        return self.op(eng, lambda e: e.reciprocal(o_, i_), self._bufs(in_), self._bufs(out))

    def _init_sems(self):
        if hasattr(self, "sems"):
            return
        nc = self.nc
        self.sems = {k: self.top.enter_context(nc.semaphore(f"s_{k}")) for k in self.E}
        self.dsems = {k: [self.top.enter_context(nc.semaphore(f"d_{k}{i}")) for i in range(self.NSLOT)] for k in ("sp", "pool")}
        self.cnt = {k: 0 for k in self.E}
        self.slotcnt = {k: [0] * self.NSLOT for k in ("sp", "pool")}

    def flush(self, final_bufs=()):
        nc = self.nc
        self._init_sems()
        sems, dsems = self.sems, self.dsems
        fin_deps = []
        for b in final_bufs:
            fin_deps.extend(b.w.values())
        for d in fin_deps:
            d.needed = True
        for k in self.E:
            if self.ops[k]:
                last = [o for o in self.ops[k] if not o.is_dma]
                if last:
                    last[-1].needed = True
        for k in self.E:
            for o in self.ops[k]:
                if o.val is not None:
                    continue
                if o.is_dma:
                    s = o.slot[1]
                    self.slotcnt[k][s] += 16
                    o.val = self.slotcnt[k][s]
                elif o.needed:
                    self.cnt[k] += 1
                    o.val = self.cnt[k]
        fin_c = dict(self.cnt)
        fin_d = {k: list(v) for k, v in self.slotcnt.items()}

        def semof(o):
            if o.is_dma:
                return ("d", o.slot), dsems[o.slot[0]][o.slot[1]]
            return ("c", o.eng), sems[o.eng]

        def make(k):
            def body(e):
                seen = {}

                def wait(o):
                    if o.val is None:
                        return
                    key, s = semof(o)
                    if seen.get(key, 0) >= o.val:
                        return
                    seen[key] = o.val
                    e.wait_ge(s, o.val)

                for o in self.ops[k]:
                    for d in o.deps:
                        wait(d)
                    if o.is_dma:
                        key, s = semof(o)
                        if o.val > 16 and seen.get(key, 0) < o.val - 16:
                            seen[key] = o.val - 16
                            e.wait_ge(s, o.val - 16)
                        o.fn(e).then_inc(s, 16)
                    else:
                        ins = o.fn(e)
                        if o.needed:
                            ins.then_inc(sems[k], 1)
                for k2 in self.E:
                    if fin_c[k2] > 0 and seen.get(("c", k2), 0) < fin_c[k2]:
                        e.wait_ge(sems[k2], fin_c[k2])
                for q in fin_d:
                    for i, v in enumerate(fin_d[q]):
                        if v > 0 and seen.get(("d", (q, i)), 0) < v:
                            e.wait_ge(dsems[q][i], v)

            return body

        with nc.Block() as block:
            deco = {"pe": block.tensor, "act": block.scalar, "dve": block.vector, "pool": block.gpsimd, "sp": block.sync}
            for k in self.E:
                deco[k](make(k))
        self.ops = {k: [] for k in self.E}

    def emit(self, final_bufs=()):
        self.flush(final_bufs)

    @contextlib.contextmanager
    def scope(self):
        old = self.ctx
        with contextlib.ExitStack() as st:
            self.ctx = st
            yield
            self.flush()
        self.ctx = old


D = 2048
NKC = 16
EPS = 1e-6


class Pool_:
    def __init__(self, P, n, shape, dt, kind="sb", name="pool", multi=False):
        if kind == "sb":
            self.bufs = [P.sb(shape, dt, f"{name}{i}", multi) for i in range(n)]
        else:
            self.bufs = [P.ps(shape, dt, f"{name}{i}") for i in range(n)]
        self.i = 0

    def get(self):
        b = self.bufs[self.i % len(self.bufs)]
        self.i += 1
        return b


def dview(buf, offset, dims):
    return buf.v(bass.AP(tensor=buf.t.tensor, offset=offset, ap=[list(d) for d in dims]))


def linear(P, wpool, wsrc, cc, nkc, rhs_fn, n, pspool, weng="pool"):
    wb = wpool.get()
    P.dma(wb[:, 0:nkc, :], wsrc[cc], eng=weng)
    ps = pspool.get()
    for n0 in range(0, n, 512):
        n1 = min(n, n0 + 512)
        for kc in range(nkc):
            P.mm(ps[:, n0:n1], wb[:, kc, :], rhs_fn(kc)[:, n0:n1], start=(kc == 0), stop=(kc == nkc - 1))
    return ps


def rmsnorm_mod(P, S, xt, hT, n, A, B, j, out_fn=None, post=None):
    sq = S["sq"]
    for kc in range(NKC):
        P.act(sq[:, kc, 0:n], xt[:, kc, 0:n], AF.Square)
    ps = S["ps1"].get()
    for kc in range(NKC):
        P.mm(ps[:, 0:n], S["ones_bf"][:, :], sq[:, kc, 0:n], start=(kc == 0), stop=(kc == NKC - 1))
    rs = S["rstd"]
    P.act(rs[:, 0:n], ps[:, 0:n], AF.Sqrt, bias=S["epsb"][:, 0:1], scale=1.0 / D)
    P.recip(rs[:, 0:n], rs[:, 0:n])
    tmp = S["tmp"]
    for kc in range(NKC):
        P.tt("dve", tmp[:, 0:n], xt[:, kc, 0:n], rs[:, 0:n], ALU.mult)
        o = hT[:, kc, 0:n] if out_fn is None else out_fn(kc)
        P.ts("dve", o, tmp[:, 0:n], A[:, kc, j:j + 1], B[:, kc, j:j + 1], ALU.mult, ALU.add)
        if post is not None:
            post(kc, o)


def mod_vectors(P, S, cc_d, adaw_d, adab_d, nvec):
    cs = P.sb([128, NKC, 2], F32, "cs_s")
    P.dma(cs[:, :, :], cc_d[:, :, :])
    sg = P.sb([128, NKC, 2], F32, "csg")
    P.act(sg[:, :, :], cs[:, :, :], AF.Sigmoid)
    P.tt("dve", cs[:, :, :], cs[:, :, :], sg[:, :, :], ALU.mult)
    ab = P.sb([128, nvec * NKC], F32, "adab_s")
    P.dma(ab[:, :], adab_d[:, :])
    mod = P.sb([128, nvec * NKC, 2], F32, "mod_s")
    wp = S["wpool_f32"]
    for c in range(nvec * NKC):
        wb = wp.get()
        P.dma(wb[:, :, :], adaw_d[c], eng="sp")
        ps = S["ps1"].get()
        for kc in range(NKC):
            P.mm(ps[:, 0:2], wb[:, kc, :], cs[:, kc, :], start=(kc == 0), stop=(kc == NKC - 1))
        P.ts("dve", mod[:, c, :], ps[:, 0:2], ab[:, c:c + 1], None, ALU.add)
    return mod


def common_state(P, nmax):
    S = {}
    S["ones_bf"] = P.sb([128, 128], BF16, "ones_bf")
    P.memset("dve", S["ones_bf"][:, :], 1.0)
    S["epsb"] = P.sb([128, 1], F32, "epsb")
    P.memset("dve", S["epsb"][:, :], EPS)
    S["sq"] = P.sb([128, NKC, nmax], BF16, "sq", multi=True)
    S["rstd"] = P.sb([128, nmax], F32, "rstd")
    S["tmp"] = P.sb([128, nmax], F32, "tmpn")
    S["ps1"] = Pool_(P, 2, [128, 512], F32, "ps", "ps1_")
    S["wpool_f32"] = Pool_(P, 2, [128, NKC, 128], F32, "sb", "wf32_")
    return S

NTOK = 4160
PCOLS = 5632
BLKS = [(i * 512, 512, 0) for i in range(8)] + [(4096, 64, 1)]


def ext(nc, name, shape, dt, out=False):
    return Buf(nc.dram_tensor(name, list(shape), dt, kind="ExternalOutput" if out else "ExternalInput").ap(), name, multi=True)


def load_block(P, xt, xT_d, t0, n):
    for kc in range(NKC):
        P.dma(xt[:, kc, 0:n], xT_d[kc, :, t0:t0 + n], eng="sp")


def build_A():
    nc = bass.Bass("TRN2", target_bir_lowering=False)
    ctx = contextlib.ExitStack()
    with ctx:
        P = Prog(nc, ctx)
        xT_d = ext(nc, "xT", [NKC, 128, NTOK], F32)
        cc_d = ext(nc, "cc", [128, NKC, 2], F32)
        adaw_d = ext(nc, "adaw", [2 * NKC, 128, NKC, 128], F32)
        adab_d = ext(nc, "adab", [128, 2 * NKC], F32)
        nw_d = ext(nc, "n1w", [128, NKC], F32)
        win_d = ext(nc, "win", [PCOLS // 128, 128, NKC, 128], F32)
        pT_d = ext(nc, "pT", [PCOLS // 128, 128, NTOK], BF16, out=True)
        S = common_state(P, 512)
        mod = mod_vectors(P, S, cc_d, adaw_d, adab_d, 2)
        nw = P.sb([128, NKC], F32, "nw_s")
        P.dma(nw[:, :], nw_d[:, :])
        A1 = P.sb([128, NKC, 2], F32, "A1")
        for j in range(2):
            P.ts("dve", A1[:, :, j], mod[:, NKC:2 * NKC, j], 1.0, None, ALU.add)
            P.tt("dve", A1[:, :, j], A1[:, :, j], nw[:, :], ALU.mult)
        xpool = Pool_(P, 2, [128, NKC, 512], F32, "sb", "xt", multi=True)
        hpool = Pool_(P, 2, [128, NKC, 512], BF16, "sb", "hT", multi=True)
        wpool = Pool_(P, 3, [128, NKC, 128], BF16, "sb", "wb")
        pspool = Pool_(P, 4, [128, 512], F32, "ps", "psl")
        opool = Pool_(P, 3, [128, 512], BF16, "sb", "ost")
        for (t0, n, j) in BLKS:
            xt = xpool.get()
            load_block(P, xt, xT_d, t0, n)
            hT = hpool.get()
            rmsnorm_mod(P, S, xt, hT, n, A1, mod, j)
            for cc in range(PCOLS // 128):
                ps = linear(P, wpool, win_d, cc, NKC, lambda kc: hT[:, kc, :], n, pspool)
                ot = opool.get()
                P.act(ot[:, 0:n], ps[:, 0:n], AF.Copy)
                P.dma(pT_d[cc, :, t0:t0 + n], ot[:, 0:n], eng="sp")
        P.emit(final_bufs=[pT_d])
    return nc


FFH = 5632
NHC = FFH // 128


def build_C():
    nc = bass.Bass("TRN2", target_bir_lowering=False)
    ctx = contextlib.ExitStack()
    with ctx:
        P = Prog(nc, ctx)
        xT_d = ext(nc, "xT", [NKC, 128, NTOK], F32)
        yT_d = ext(nc, "yT", [NKC, 128, NTOK], BF16)
        cc_d = ext(nc, "cc", [128, NKC, 2], F32)
        adaw_d = ext(nc, "adaw", [6 * NKC, 128, NKC, 128], F32)
        adab_d = ext(nc, "adab", [128, 6 * NKC], F32)
        n1_d = ext(nc, "n1w", [128, NKC], F32)
        n2_d = ext(nc, "n2w", [128, NKC], F32)
        fn_d = ext(nc, "fnw", [128, NKC], F32)
        wg_d = ext(nc, "wg", [4 * NKC, 128, NKC, 128], F32)
        wbr_d = ext(nc, "wbr", [4 * NKC, 128, 4, 128], F32)
        wo_d = ext(nc, "wo", [NKC, 128, NKC, 128], F32)
        w13_d = ext(nc, "w13", [2 * NHC, 128, NKC, 128], F32)
        w2_d = ext(nc, "w2", [NKC, 128, NHC, 128], F32)
        xo_d = ext(nc, "xo", [NKC, 128, NTOK], F32, out=True)
        fo_d = ext(nc, "fo", [NKC, 128, NTOK], F32, out=True)
        S = common_state(P, 512)
        mod = mod_vectors(P, S, cc_d, adaw_d, adab_d, 6)
        nws = P.sb([128, 3, NKC], F32, "nws")
        P.dma(nws[:, 0, :], n1_d[:, :]); P.dma(nws[:, 1, :], n2_d[:, :]); P.dma(nws[:, 2, :], fn_d[:, :])
        A1 = P.sb([128, NKC, 2], F32, "A1")
        A2 = P.sb([128, NKC, 2], F32, "A2")
        A3 = P.sb([128, NKC, 2], F32, "A3")
        Z3 = P.sb([128, NKC, 2], F32, "Z3")
        P.memset("dve", Z3[:, :, :], 0.0)
        for j in range(2):
            P.ts("dve", A1[:, :, j], mod[:, NKC:2 * NKC, j], 1.0, None, ALU.add)
            P.tt("dve", A1[:, :, j], A1[:, :, j], nws[:, 0, :], ALU.mult)
            P.ts("dve", A2[:, :, j], mod[:, 4 * NKC:5 * NKC, j], 1.0, None, ALU.add)
            P.tt("dve", A2[:, :, j], A2[:, :, j], nws[:, 1, :], ALU.mult)
            P.copy("dve", A3[:, :, j], nws[:, 2, :])
        xpool = Pool_(P, 1, [128, NKC, 512], F32, "sb", "xt", multi=True)
        hT = P.sb([128, NKC, 512], BF16, "hT", multi=True)
        yT = P.sb([128, NKC, 512], BF16, "yT", multi=True)
        mT = P.sb([128, NKC, 512], BF16, "mT", multi=True)
        uT = P.sb([128, NHC, 512], BF16, "uT", multi=True)
        fpool = Pool_(P, 2, [128, 512], F32, "sb", "fst")
        wpool = Pool_(P, 3, [128, NHC, 128], BF16, "sb", "wb")
        pspool = Pool_(P, 4, [128, 512], F32, "ps", "psl")
        sgp = Pool_(P, 2, [128, 512], BF16, "sb", "sg")
        prp = Pool_(P, 2, [128, 512], F32, "sb", "pr")
        acc = P.sb([128, 512], F32, "acc")
        for (t0, n, j) in BLKS:
            xt = xpool.get()
            load_block(P, xt, xT_d, t0, n)
            for c in range(NKC):
                P.dma(yT[:, c, 0:n], yT_d[c, :, t0:t0 + n], eng="sp")
            rmsnorm_mod(P, S, xt, hT, n, A1, mod, j)
            for cc in range(NKC):
                for i in range(4):
                    psg = linear(P, wpool, wg_d, i * NKC + cc, NKC, lambda kc: hT[:, kc, :], n, pspool)
                    sg = sgp.get()
                    P.act(sg[:, 0:n], psg[:, 0:n], AF.Sigmoid)
                    psb = linear(P, wpool, wbr_d, i * NKC + cc, 4, lambda kc: yT[:, i * 4 + kc, :], n, pspool)
                    if i == 0:
                        P.tt("dve", acc[:, 0:n], psb[:, 0:n], sg[:, 0:n], ALU.mult)
                    else:
                        pr = prp.get()
                        P.tt("dve", pr[:, 0:n], psb[:, 0:n], sg[:, 0:n], ALU.mult)
                        if i < 3:
                            P.tt("pool", acc[:, 0:n], acc[:, 0:n], pr[:, 0:n], ALU.add)
                        else:
                            P.tt("pool", mT[:, cc, 0:n], acc[:, 0:n], pr[:, 0:n], ALU.add)
            for cc in range(NKC):
                ps = linear(P, wpool, wo_d, cc, NKC, lambda kc: mT[:, kc, :], n, pspool)
                P.stt(xt[:, cc, 0:n], ps[:, 0:n], mod[:, 2 * NKC + cc, j:j + 1], xt[:, cc, 0:n], ALU.mult, ALU.add)
            rmsnorm_mod(P, S, xt, hT, n, A2, mod[:, 3 * NKC:4 * NKC, :], j)
            for hc in range(NHC):
                psa = linear(P, wpool, w13_d, hc, NKC, lambda kc: hT[:, kc, :], n, pspool)
                sg = sgp.get()
                P.act(sg[:, 0:n], psa[:, 0:n], AF.Silu)
                psb = linear(P, wpool, w13_d, NHC + hc, NKC, lambda kc: hT[:, kc, :], n, pspool)
                P.tt("dve", uT[:, hc, 0:n], psb[:, 0:n], sg[:, 0:n], ALU.mult)
            for cc in range(NKC):
                ps = linear(P, wpool, w2_d, cc, NHC, lambda kc: uT[:, kc, :], n, pspool)
                P.stt(xt[:, cc, 0:n], ps[:, 0:n], mod[:, 5 * NKC + cc, j:j + 1], xt[:, cc, 0:n], ALU.mult, ALU.add)
            for kc in range(NKC):
                P.dma(xo_d[kc, :, t0:t0 + n], xt[:, kc, 0:n], eng="sp")
            rmsnorm_mod(P, S, xt, None, n, A3, Z3, j, out_fn=lambda kc: fpool.get()[:, 0:n],
                        post=lambda kc, o: P.dma(fo_d[kc, :, t0:t0 + n], o, eng="sp"))
        P.emit(final_bufs=[xo_d, fo_d])
    return nc


NL = 16384
NKT = 130
SCALE = 128 ** -0.5
TWO_PI = 6.283185307179586


def norm_rope(P, S, src, n, nw_col, cs_d, t0, PmT, out, rope, sbp):
    sq = sbp["sq"].get()
    P.act(sq[:, 0:n], src, AF.Square)
    ps = S["ps1"].get()
    P.mm(ps[:, 0:n], S["ones_bf"][:, :], sq[:, 0:n])
    rs = sbp["rs"].get()
    P.act(rs[:, 0:n], ps[:, 0:n], AF.Sqrt, bias=S["epsb"][:, 0:1], scale=1.0 / 128)
    P.recip(rs[:, 0:n], rs[:, 0:n])
    if not rope:
        P.stt(out, src, nw_col, rs[:, 0:n], ALU.mult, ALU.mult)
        return
    kn = sbp["kn"].get()
    P.stt(kn[:, 0:n], src, nw_col, rs[:, 0:n], ALU.mult, ALU.mult)
    cs = sbp["cs"].get()
    P.dma(cs[:, 0, 0:n], cs_d[0, :, t0:t0 + n])
    P.dma(cs[:, 1, 0:n], cs_d[1, :, t0:t0 + n])
    ps2 = S["ps1"].get()
    P.mm(ps2[:, 0:n], PmT[:, :], kn[:, 0:n])
    t1 = sbp["t1"].get()
    P.tt("pool", t1[:, 0:n], kn[:, 0:n], cs[:, 0, 0:n], ALU.mult)
    t2 = sbp["t2"].get()
    P.tt("dve", t2[:, 0:n], ps2[:, 0:n], cs[:, 1, 0:n], ALU.mult)
    P.tt("dve", out, t1[:, 0:n], t2[:, 0:n], ALU.add)


def attn_core(P, S, KT, Vt, key_tiles, qr, n, pss, ps_o, ps_l, epool, out_d, o0, opool, bias=None):
    nk = len(key_tiles)
    for i, (kv_, vv_) in enumerate(key_tiles):
        ps = pss.get()
        P.mm(ps[:, 0:n], kv_, qr)
        e = epool.get()
        if bias is not None and bias[i] is not None:
            tb = S["tb"].get()
            P.stt(tb[:, 0:n], ps[:, 0:n], SCALE, bias[i], ALU.mult, ALU.add)
            P.act(e[:, 0:n], tb[:, 0:n], AF.Exp)
        else:
            P.act(e[:, 0:n], ps[:, 0:n], AF.Exp, scale=SCALE)
        P.mm(ps_o[:, 0:n], vv_, e[:, 0:n], start=(i == 0), stop=(i == nk - 1))
        P.mm(ps_l[:, 0:n], S["ones_bf"][:, :], e[:, 0:n], start=(i == 0), stop=(i == nk - 1))
    rl = S["rl"].get()
    P.recip(rl[:, 0:n], ps_l[:, 0:n])
    o = opool.get()
    P.tt("dve", o[:, 0:n], ps_o[:, 0:n], rl[:, 0:n], ALU.mult)
    P.dma(out_d[:, o0:o0 + n], o[:, 0:n])


def gqa_mixer(P, S, nc, D_):
    with P.scope():
        KT = P.sb([128, NL + 256], BF16, "KT", multi=True)
        Vt = P.sb([128, NKT, 128], BF16, "Vt")
        P.dma(Vt[:, 0:65, :], D_["gv"][:, 0:65, :]); P.dma(Vt[:, 65:NKT, :], D_["gv"][:, 65:NKT, :])
        PmT = P.sb([128, 128], BF16, "PmT"); P.dma(PmT[:, :], D_["PmT"][:, :])
        gnw = P.sb([128, 2], F32, "gnw"); P.dma(gnw[:, :], D_["gnw"][:, :])
        sbp = {"sq": Pool_(P, 2, [128, 512], BF16, "sb", "gsq"), "rs": Pool_(P, 2, [128, 512], F32, "sb", "grs"),
               "kn": Pool_(P, 2, [128, 512], BF16, "sb", "gkn"), "cs": Pool_(P, 2, [128, 2, 512], F32, "sb", "gcs", multi=True),
               "t1": Pool_(P, 2, [128, 512], F32, "sb", "gt1"), "t2": Pool_(P, 2, [128, 512], F32, "sb", "gt2")}
        inp = Pool_(P, 2, [128, 512], BF16, "sb", "gin")
        for kb in range(33):
            t0 = kb * 512
            n = 512 if kb < 32 else 256
            x = inp.get()
            P.dma(x[:, 0:n], D_["gk"][:, t0:t0 + n])
            norm_rope(P, S, x[:, 0:n], n, gnw[:, 1:2], D_["rope_cs"], t0, PmT, KT[:, t0:t0 + n], kb < 32, sbp)
        pss = Pool_(P, 3, [128, 512], F32, "ps", "gps")
        ps_o = P.ps([128, 512], F32, "gpo"); ps_l = P.ps([128, 512], F32, "gpl")
        epool = Pool_(P, 3, [128, 512], BF16, "sb", "ge")
        opool = Pool_(P, 2, [128, 512], BF16, "sb", "go")
        qrp = Pool_(P, 2, [128, 512], BF16, "sb", "gqr")
        S["rl"] = Pool_(P, 2, [128, 512], F32, "sb", "grl")
        for qb in range(33):
            t0 = qb * 512
            n = 512 if qb < 32 else 256
            x = inp.get()
            P.dma(x[:, 0:n], D_["gq"][:, t0:t0 + n])
            qr = qrp.get()
            norm_rope(P, S, x[:, 0:n], n, gnw[:, 0:1], D_["rope_cs"], t0, PmT, qr[:, 0:n], qb < 32, sbp)
            tiles = range(NKT) if qb < 32 else range(128, NKT)
            kt = [(KT[:, i * 128:(i + 1) * 128], Vt[:, i, :]) for i in tiles]
            attn_core(P, S, KT, Vt, kt, qr[:, 0:n], n, pss, ps_o, ps_l, epool, D_["yg"], t0, opool)


def na_mixer(P, S, nc, D_):
    with P.scope():
        KT = P.sb([128, NL + 256], BF16, "nKT")
        P.dma(KT[:, 0:8192], D_["nk"][:, 0:8192]); P.dma(KT[:, 8192:NL + 256], D_["nk"][:, 8192:NL + 256])
        Ve = P.sb([128, NKT, 128], BF16, "nVe"); Vo = P.sb([128, 127, 128], BF16, "nVo")
        P.dma(Ve[:, 0:65, :], D_["nve"][:, 0:65, :]); P.dma(Ve[:, 65:NKT, :], D_["nve"][:, 65:NKT, :])
        P.dma(Vo[:, 0:64, :], D_["nvo"][:, 0:64, :]); P.dma(Vo[:, 64:127, :], D_["nvo"][:, 64:127, :])
        nb = P.sb([128, 9, 4, 64], F32, "nbias")
        for c in range(9):
            P.dma(nb[:, c, :, :], D_["nbias"][c])
        pss = Pool_(P, 3, [128, 512], F32, "ps", "nps")
        ps_o = P.ps([128, 512], F32, "npo"); ps_l = P.ps([128, 512], F32, "npl")
        epool = Pool_(P, 3, [128, 512], BF16, "sb", "ne")
        opool = Pool_(P, 2, [128, 512], BF16, "sb", "no")
        S["rl"] = Pool_(P, 2, [128, 512], F32, "sb", "nrl")
        S["tb"] = Pool_(P, 3, [128, 512], F32, "sb", "ntb")
        qp = Pool_(P, 2, [128, 2048], BF16, "sb", "nq")
        for r in range(256):
            if r % 32 == 0:
                qt = qp.get()
                P.dma(qt[:, :], D_["nq"][:, r * 64:r * 64 + 2048])
            qr = qt[:, (r % 32) * 64:(r % 32) * 64 + 64]
            r0 = min(max(r - 4, 0), 248)
            cls = r if r < 4 else (4 if r < 252 else r - 247)
            kt, bs = [], []
            for i in range(4):
                tk = r0 * 64 + i * 128
                vv = Ve[:, tk // 128, :] if r0 % 2 == 0 else Vo[:, (tk - 64) // 128, :]
                kt.append((KT[:, tk:tk + 128], vv)); bs.append(nb[:, cls, i, :])
            for i in (128, 129):
                kt.append((KT[:, i * 128:(i + 1) * 128], Ve[:, i, :])); bs.append(None)
            attn_core(P, S, KT, None, kt, qr, 64, pss, ps_o, ps_l, epool, D_["yn"], r * 64, opool, bias=bs)
        qt = qp.get()
        P.dma(qt[:, 0:256], D_["nq"][:, NL:NL + 256])
        kt = [(KT[:, i * 128:(i + 1) * 128], Ve[:, i, :]) for i in (128, 129)]
        attn_core(P, S, KT, None, kt, qt[:, 0:256], 256, pss, ps_o, ps_l, epool, D_["yn"], NL, opool)


def ret_mixer(P, S, nc, D_):
    NT = NL + 256
    with P.scope():
        tab = P.sb([128, 4, 128], F32, "rtab"); P.dma(tab[:, :, :], D_["rtab"][:, :, :])
        pcol = P.sb([128, 2], F32, "rpcol"); P.dma(pcol[:, :], D_["rpcol"][:, :])
        qdt = P.sb([64, 2, 128], F32, "rqdt"); P.dma(qdt[:, :, :], D_["rqd"][:, :, :])
        ld = P.sb([128, 2], F32, "rld"); P.dma(ld[:, :], D_["rld"][:, :])
        gnw = P.sb([128, 1], F32, "rgnw"); P.dma(gnw[:, :], D_["rgnw"][:, :])
        Pm = P.sb([64, 64], BF16, "rPm"); P.dma(Pm[:, :], D_["Pm64T"][:, :])
        lg = P.sb([128, 2], F32, "rlg")
        P.ts("dve", lg[:, :], ld[:, :], -1.0, None, ALU.mult)
        P.tt("dve", lg[:, :], lg[:, :], ld[:, :], ALU.min)
        ksc = 64 ** -0.5
        intra = P.sb([128, 2, 128], BF16, "rintra", multi=True)
        kdec = P.sb([128, 2], F32, "rkdec", multi=True)
        qdec = P.sb([64, 2, 128], F32, "rqdec", multi=True)
        gC = P.sb([64, 2], F32, "rgC", multi=True)
        e1 = P.sb([128, 128], F32, "re1")
        for dr in range(2):
            P.act(e1[:, :], tab[:, 2 * dr, :], AF.Exp, scale=lg[:, dr:dr + 1])
            P.stt(intra[:, dr, :], e1[:, :], ksc, tab[:, 2 * dr + 1, :], ALU.mult, ALU.mult)
            P.act(kdec[:, dr:dr + 1], pcol[:, dr:dr + 1], AF.Exp, scale=lg[:, dr:dr + 1])
            P.act(qdec[:, dr, :], qdt[:, dr, :], AF.Exp, scale=lg[0:64, dr:dr + 1])
            P.act(gC[:, dr:dr + 1], lg[0:64, dr:dr + 1], AF.Exp, scale=128.0)
        P.ts("dve", kdec[:, :], kdec[:, :], ksc, None, ALU.mult)
        vt = P.sb([128, NKT, 128], BF16, "rvt"); P.dma(vt[:, 0:65, :], D_["rvt"][:, 0:65, :]); P.dma(vt[:, 65:NKT, :], D_["rvt"][:, 65:NKT, :])
        SP = P.sb([64, 2, NKT, 128], BF16, "rSP", multi=True)
        St = P.sb([64, 2, 128], F32, "rS")
        P.memset("dve", St[:, :, :], 0.0)
        order = [[128, 129] + list(range(128)), [129, 128] + list(range(127, -1, -1))]
        with P.scope():
            ktr = P.sb([128, NKT, 64], BF16, "rktr", multi=True)
            with P.scope():
                kt = P.sb([128, NKT, 64], BF16, "rkt"); P.dma(kt[:, :, :], D_["rkt"][:, :, :])
                ct = P.sb([128, 2, NKT, 32], F32, "rct", multi=True)
                P.dma(ct[:, 0, :, :], D_["rropet"][0]); P.dma(ct[:, 1, :, :], D_["rropet"][1])
                ta = P.sb([128, NKT, 32], F32, "rta"); tb_ = P.sb([128, NKT, 32], F32, "rtb")
                x1, x2 = kt[:, :, 0:32], kt[:, :, 32:64]
                P.tt("dve", ta[:, :, :], x1, ct[:, 0, :, :], ALU.mult); P.tt("pool", tb_[:, :, :], x2, ct[:, 1, :, :], ALU.mult)
                P.tt("dve", ktr[:, :, 0:32], ta[:, :, :], tb_[:, :, :], ALU.subtract)
                P.tt("dve", ta[:, :, :], x1, ct[:, 1, :, :], ALU.mult); P.tt("pool", tb_[:, :, :], x2, ct[:, 0, :, :], ALU.mult)
                P.tt("dve", ktr[:, :, 32:64], ta[:, :, :], tb_[:, :, :], ALU.add)
            kd = P.sb([128, NKT, 64], BF16, "rkd")
            KV = P.sb([64, NKT, 128], BF16, "rKV", multi=True)
            pskv = Pool_(P, 2, [128, 512], F32, "ps", "rpk")
            for dr in range(2):
                P.ts("dve", kd[:, :, :], ktr[:, :, :], kdec[:, dr:dr + 1], None, ALU.mult)
                for c0 in range(0, NKT, 4):
                    ps = pskv.get()
                    cn = min(4, NKT - c0)
                    for i in range(cn):
                        P.mm(ps[0:64, i * 128:(i + 1) * 128], kd[:, c0 + i, :], vt[:, c0 + i, :])
                    P.act(KV[:, c0:c0 + cn, :], ps[0:64, 0:cn * 128].re("p (a b) -> p a b", b=128), AF.Copy)
                for step in range(NKT):
                    c = order[dr][step]
                    P.copy("pool", SP[:, dr, c, :], St[:, dr, :])
                    P.stt(St[:, dr, :], St[:, dr, :], gC[:, dr:dr + 1], KV[:, c, :], ALU.mult, ALU.add)
        cs = P.sb([64, 2, 2048], F32, "rcs", multi=True)
        qin = Pool_(P, 2, [64, 2, 2048], BF16, "sb", "rqk", multi=True)
        qr = P.sb([64, 2, 2048], BF16, "rqr", multi=True)
        ta = P.sb([64, 2048], F32, "rta2"); tb_ = P.sb([64, 2048], F32, "rtb2")
        gin = Pool_(P, 2, [128, 512], BF16, "sb", "rgin")
        pss = Pool_(P, 2, [128, 512], F32, "ps", "rps")
        pso = Pool_(P, 2, [128, 512], F32, "ps", "rpo")
        smp = Pool_(P, 4, [128, 128], BF16, "sb", "rsm")
        qdp = Pool_(P, 4, [64, 128], BF16, "sb", "rqd")
        sqp = Pool_(P, 2, [128, 512], BF16, "sb", "rsq"); rsp = Pool_(P, 2, [128, 512], F32, "sb", "rrs")
        sgp = Pool_(P, 2, [128, 512], F32, "sb", "rsg"); t3p = Pool_(P, 2, [128, 512], F32, "sb", "rt3")
        op = Pool_(P, 2, [128, 512], BF16, "sb", "ro")
        for sb_ in range(0, NT, 2048):
            n = min(2048, NT - sb_)
            qk = qin.get()
            P.dma(qk[:, 0, 0:n], D_["rq"][:, sb_:sb_ + n]); P.dma(qk[:, 1, 0:n], D_["rk"][:, sb_:sb_ + n])
            P.dma(cs[:, 0, 0:n], D_["rrope"][0, :, sb_:sb_ + n]); P.dma(cs[:, 1, 0:n], D_["rrope"][1, :, sb_:sb_ + n])
            for w in range(2):
                for n0 in range(0, n, 512):
                    n1 = min(n, n0 + 512)
                    ps = pss.get()
                    P.mm(ps[0:64, 0:n1 - n0], Pm[:, :], qk[:, w, n0:n1])
                    P.tt("dve", tb_[:, n0:n1], ps[0:64, 0:n1 - n0], cs[:, 1, n0:n1], ALU.mult)
                P.tt("pool", ta[:, 0:n], qk[:, w, 0:n], cs[:, 0, 0:n], ALU.mult)
                P.tt("dve", qr[:, w, 0:n], ta[:, 0:n], tb_[:, 0:n], ALU.add)
            for g0 in range(0, n, 512):
                gn = min(512, n - g0)
                po = pso.get()
                gt = gin.get()
                P.dma(gt[:, 0:gn], D_["rg"][:, sb_ + g0:sb_ + g0 + gn])
                for c0 in range(0, gn, 128):
                    ch = (sb_ + g0 + c0) // 128
                    lo = g0 + c0
                    ps = pss.get()
                    P.mm(ps[:, 0:128], qr[:, 1, lo:lo + 128], qr[:, 0, lo:lo + 128])
                    first = True
                    for dr in range(2):
                        sm = smp.get()
                        P.tt("dve", sm[:, :], ps[:, 0:128], intra[:, dr, :], ALU.mult)
                        qd = qdp.get()
                        P.tt("pool", qd[:, :], qr[:, 0, lo:lo + 128], qdec[:, dr, :], ALU.mult)
                        P.mm(po[:, c0:c0 + 128], vt[:, ch, :], sm[:, :], start=first, stop=False)
                        first = False
                        P.mm(po[:, c0:c0 + 128], SP[:, dr, ch, :], qd[:, :], start=False, stop=(dr == 1))
                sq = sqp.get()
                P.act(sq[:, 0:gn], po[:, 0:gn], AF.Square)
                ps = pss.get()
                P.mm(ps[:, 0:gn], S["ones_bf"][:, :], sq[:, 0:gn])
                rs = rsp.get()
                P.act(rs[:, 0:gn], ps[:, 0:gn], AF.Sqrt, bias=S["epsb"][:, 0:1], scale=1.0 / 128)
                P.recip(rs[:, 0:gn], rs[:, 0:gn])
                sg = sgp.get()
                P.act(sg[:, 0:gn], gt[:, 0:gn], AF.Silu)
                t3 = t3p.get()
                P.stt(t3[:, 0:gn], po[:, 0:gn], gnw[:, 0:1], rs[:, 0:gn], ALU.mult, ALU.mult)
                o = op.get()
                P.tt("dve", o[:, 0:gn], t3[:, 0:gn], sg[:, 0:gn], ALU.mult)
                P.dma(D_["yr"][:, sb_ + g0:sb_ + g0 + gn], o[:, 0:gn])


PI = 3.141592653589793


def hyena_filter(P, S, D_, L, feat_d, hf_d, rnB, col0):
    with P.scope():
        w1 = P.sb([33, 64], F32, "fw1"); P.dma(w1[:, :], D_["fw1"][:, :])
        w2 = P.sb([64, 64], F32, "fw2"); P.dma(w2[:, :], D_["fw2"][:, :])
        w3 = P.sb([64, 2, 128], F32, "fw3"); P.dma(w3[:, :, :], D_["fw3"][:, :, :])
        fsc = P.sb([64, 4], F32, "fsc"); P.dma(fsc[:, :], D_["fsc"][:, :])
        ndl = P.sb([128, 1], F32, "ndl"); P.dma(ndl[:, :], D_["ndl"][:, :])
        CS = min(512, L)
        nch = (2 * L) // CS
        ss = P.sb([128, nch], F32, "fss", multi=True)
        ftp = Pool_(P, 2, [33, 512], F32, "sb", "fft")
        tnp = Pool_(P, 2, [128, 512], F32, "sb", "ftn")
        zp = Pool_(P, 4, [64, 512], F32, "sb", "fz")
        mp = Pool_(P, 4, [64, 512], F32, "sb", "fm")
        dcp = Pool_(P, 2, [128, 512], F32, "sb", "fdc")
        hbp = Pool_(P, 2, [128, 512], BF16, "sb", "fhb")
        sqj = P.sb([128, 512], F32, "fsqj")
        psp = Pool_(P, 2, [128, 512], F32, "ps", "fps")
        mpi = P.sb([64, 1], F32, "fmpi"); P.memset("dve", mpi[:, :], 0.0)

        def sin_layer(ps, bcol, fcol):
            z = zp.get()
            P.ts("dve", z[:, 0:CS], ps[0:64, 0:CS], fsc[:, bcol:bcol + 1], fsc[:, fcol:fcol + 1], ALU.add, ALU.mult)
            m1 = mp.get(); m2 = mp.get()
            P.ts("pool", m1[:, 0:CS], z[:, 0:CS], PI, -TWO_PI, ALU.is_gt, ALU.mult)
            P.ts("pool", m2[:, 0:CS], z[:, 0:CS], -PI, TWO_PI, ALU.is_lt, ALU.mult)
            P.tt("dve", m1[:, 0:CS], m1[:, 0:CS], m2[:, 0:CS], ALU.add)
            P.tt("dve", z[:, 0:CS], z[:, 0:CS], m1[:, 0:CS], ALU.add)
            P.act(z[:, 0:CS], z[:, 0:CS], AF.Sin)
            return z

        for ch in range(nch):
            x0 = ch * CS
            ft = ftp.get(); P.dma(ft[:, 0:CS], feat_d[:, x0:x0 + CS])
            tn = tnp.get()
            P.dma(tn[:, 0:CS], dview(feat_d, x0, [[0, 128], [1, CS]]))
            ps = psp.get()
            P.mm(ps[0:64, 0:CS], w1[:, :], ft[:, 0:CS])
            z1 = sin_layer(ps, 0, 1)
            ps = psp.get()
            P.mm(ps[0:64, 0:CS], w2[:, :], z1[:, 0:CS])
            z2 = sin_layer(ps, 2, 3)
            ps = psp.get()
            dirn = 1 if x0 < L else 0
            P.mm(ps[:, 0:CS], w3[:, dirn, :], z2[:, 0:CS])
            dc = dcp.get()
            P.act(dc[:, 0:CS], tn[:, 0:CS], AF.Exp, scale=ndl[:, 0:1])
            hb = hbp.get()
            P.tt("dve", hb[:, 0:CS], ps[:, 0:CS], dc[:, 0:CS], ALU.mult)
            lo = 1 if ch == 0 else 0
            P.act(sqj[:, lo:CS], hb[:, lo:CS], AF.Square, accum_out=ss[:, ch:ch + 1])
            P.dma(hf_d[:, x0:x0 + CS], hb[:, 0:CS])
        tot = P.sb([128, 1], F32, "ftot")
        P.reduce(tot[:, :], ss[:, :], ALU.add)
        P.act(tot[:, :], tot[:, :], AF.Sqrt, bias=S["epsb"][:, 0:1], scale=1.0)
        P.recip(tot[:, :], tot[:, :])
        idt = P.sb([128, 128], F32, "fid"); P.dma(idt[:, :], D_["ident"][:, :])
        dg = P.sb([128, 128], F32, "fdg")
        P.ts("dve", dg[:, :], idt[:, :], tot[:, 0:1], None, ALU.mult)
        onesf = P.sb([128, 128], F32, "fones"); P.memset("dve", onesf[:, :], 1.0)
        ps = psp.get()
        P.mm(ps[:, 0:128], onesf[:, :], dg[:, :])
        P.copy("dve", rnB[:, col0:col0 + 128], ps[:, 0:128])


def hyena_conv(P, S, D_, L, hz_d, hf_d, rnB, col0, yh_d, hw, hbias, J):
    nb = L // 128
    NB2 = 2 * nb
    W = 2 * L - 127
    with P.scope():
        hsp = Pool_(P, 2, [128, W], BF16, "sb", "hHS", multi=True)
        zp = Pool_(P, 2, [128, 12, NB2], BF16, "sb", "hz")
        up = Pool_(P, 2, [128, 4, NB2], BF16, "sb", "hu", multi=True)
        tp = Pool_(P, 4, [128, NB2], F32, "sb", "ht")
        zfp = Pool_(P, 2, [128, NB2], BF16, "sb", "hzf")
        z2p = Pool_(P, 2, [128, NB2], BF16, "sb", "hz2")
        yp = Pool_(P, 2, [128, NB2], BF16, "sb", "hy")
        psc = Pool_(P, 2, [128, 512], F32, "ps", "hpc")
        psj = Pool_(P, 2, [128, 512], F32, "ps", "hpj")
        for c in range(64):
            z = zp.get()
            P.dma(z[:, :, :], hz_d[c])
            u = up.get()
            for s in range(4):
                ws = 0 if s < 2 else s - 1
                t = tp.get()
                P.ts("dve", t[:, :], z[:, 3 * s + 0, :], hw[:, c, ws, 0:1], hw[:, c, ws, 3:4], ALU.mult, ALU.add)
                P.stt(t[:, :], z[:, 3 * s + 1, :], hw[:, c, ws, 1:2], t[:, :], ALU.mult, ALU.add)
                P.stt(u[:, s, :], z[:, 3 * s + 2, :], hw[:, c, ws, 2:3], t[:, :], ALU.mult, ALU.add)
            zin, zfl = u[:, 0, :], u[:, 1, :]
            for o in range(2):
                oc = o * 64 + c
                hs = hsp.get()
                half = W // 2
                P.dma(hs[:, 0:half], dview(hf_d, oc * 2 * L, [[1, 128], [1, half]]))
                P.dma(hs[:, half:W], dview(hf_d, oc * 2 * L + half, [[1, 128], [1, W - half]]))
                ps = psc.get()
                pv = ps[:, 0:NB2].re("p (b a) -> p b a", b=2)
                zv = zfl.re("p (b a) -> p b a", b=2)
                ds = [0] + [d for d in range(-(nb - 1), nb) if d != 0]
                for i, d in enumerate(ds):
                    lo, hi = max(0, d), min(nb - 1, nb - 1 + d)
                    c0 = L + 128 * d - 127
                    P.mm(pv[:, :, lo:hi + 1], hs[:, c0:c0 + 128], zv[:, :, lo - d:hi - d + 1],
                         start=(i == 0), stop=(i == len(ds) - 1), skip_group_check=True)
                t1 = tp.get()
                P.ts("dve", t1[:, :], zin, hbias[:, c, o:o + 1], None, ALU.mult)
                t2 = tp.get()
                P.stt(t2[:, :], ps[:, 0:NB2], rnB[:, col0 + oc:col0 + oc + 1], t1[:, :], ALU.mult, ALU.add)
                if o == 0:
                    z2 = z2p.get()
                    P.tt("dve", z2[:, :], t2[:, :], u[:, 2, :], ALU.mult)
                    pj = psj.get()
                    P.mm(pj[:, 0:NB2], J[:, :], z2[:, :])
                    zf2 = zfp.get()
                    P.copy("act", zf2[:, :], pj[:, 0:NB2])
                    zin, zfl = z2[:, :], zf2[:, :]
                else:
                    y = yp.get()
                    P.tt("dve", y[:, :], t2[:, :], u[:, 3, :], ALU.mult)
                    P.dma(yh_d[c], y[:, :])


def hyena_mixer(P, S, nc, D_):
    with P.scope():
        rnB = P.sb([128, 256], F32, "hrnB", multi=True)
        hw = P.sb([128, 64, 3, 4], F32, "hhw"); P.dma(hw[:, :, :, :], D_["hw"][:, :, :, :])
        hb = P.sb([128, 64, 2], F32, "hhb"); P.dma(hb[:, :, :], D_["hb"][:, :, :])
        J = P.sb([128, 128], BF16, "hJ"); P.dma(J[:, :], D_["J"][:, :])
        hyena_filter(P, S, D_, NL, D_["feat"], D_["hf"], rnB, 0)
        hyena_filter(P, S, D_, 256, D_["featc"], D_["hfc"], rnB, 128)
        hyena_conv(P, S, D_, NL, D_["hz"], D_["hf"], rnB, 0, D_["yh"], hw, hb, J)
        hyena_conv(P, S, D_, 256, D_["hzc"], D_["hfc"], rnB, 128, D_["yhc"], hw, hb, J)


def build_B():
    nc = bass.Bass("TRN2", target_bir_lowering=False)
    ctx = contextlib.ExitStack()
    NT = NL + 256
    with ctx:
        P = Prog(nc, ctx)
        D_ = {}
        def I(name, shape, dt): D_[name] = ext(nc, name, shape, dt)
        def O(name, shape, dt): D_[name] = ext(nc, name, shape, dt, out=True)
        I("gq", [128, NT], BF16); I("gk", [128, NT], BF16); I("gv", [128, NKT, 128], BF16)
        I("rope_cs", [2, 128, NL], F32); I("gnw", [128, 2], F32); I("PmT", [128, 128], BF16)
        I("nq", [128, NT], BF16); I("nk", [128, NT], BF16); I("nve", [128, NKT, 128], BF16); I("nvo", [128, 127, 128], BF16)
        I("nbias", [9, 128, 4, 64], F32)
        I("rq", [64, NT], BF16); I("rk", [64, NT], BF16); I("rkt", [128, NKT, 64], BF16); I("rvt", [128, NKT, 128], BF16)
        I("rg", [128, NT], BF16); I("rrope", [2, 64, NT], F32); I("rropet", [2, 128, NKT, 32], F32)
        I("Pm64T", [64, 64], BF16); I("rld", [128, 2], F32); I("rtab", [128, 4, 128], F32); I("rpcol", [128, 2], F32)
        I("rqd", [64, 2, 128], F32); I("rgnw", [128, 1], F32)
        I("hz", [64, 128, 12, 256], BF16); I("hzc", [64, 128, 12, 4], BF16); I("hw", [128, 64, 3, 4], F32); I("hb", [128, 64, 2], F32)
        I("fw1", [33, 64], F32); I("fw2", [64, 64], F32); I("fw3", [64, 2, 128], F32); I("fsc", [64, 4], F32); I("ndl", [128, 1], F32)
        I("feat", [33, 2 * NL], F32); I("featc", [33, 512], F32); I("ident", [128, 128], F32); I("J", [128, 128], BF16)
        O("yg", [128, NT], BF16); O("yn", [128, NT], BF16); O("yr", [128, NT], BF16)
        O("yh", [64, 128, 256], BF16); O("yhc", [64, 128, 4], BF16)
        D_["hf"] = P.dram("hf", [128, 2 * NL], BF16); D_["hfc"] = P.dram("hfc", [128, 512], BF16)
        S = {}
        with P.scope():
            S["ones_bf"] = P.sb([128, 128], BF16, "ones_bf"); P.memset("dve", S["ones_bf"][:, :], 1.0)
            S["epsb"] = P.sb([128, 1], F32, "epsb"); P.memset("dve", S["epsb"][:, :], EPS)
            S["ps1"] = Pool_(P, 2, [128, 512], F32, "ps", "ps1_")
            ret_mixer(P, S, nc, D_)
            na_mixer(P, S, nc, D_)
            gqa_mixer(P, S, nc, D_)
            hyena_mixer(P, S, nc, D_)
        P.flush(final_bufs=[D_["yg"], D_["yn"], D_["yr"], D_["yh"], D_["yhc"]])
    return nc
import ml_dtypes
BF = ml_dtypes.bfloat16
_PROG = {}


def _prog(name):
    if name not in _PROG:
        _PROG[name] = {"A": build_A, "B": build_B, "C": build_C}[name]()
    return _PROG[name]


def _run(name, maps):
    res = run_bass_kernel_spmd(_prog(name), maps, core_ids=list(range(8)))
    return res.results


def wlay(w):
    K, N = w.shape
    return np.ascontiguousarray(w.reshape(K // 128, 128, N // 128, 128).transpose(2, 1, 0, 3))


def vlay(v):
    return np.ascontiguousarray(v.reshape(-1, 128).T)


def tiles_tok(a, nt):
    return np.ascontiguousarray(a[:nt * 128].reshape(nt, 128, a.shape[1]).transpose(1, 0, 2))


def consts():
    f32 = np.float32
    C = {}
    t = np.arange(NL)
    inv = (f32(10000.0) ** (-np.arange(32, dtype=f32) / f32(32))).astype(f32)
    ang_r = (t // 64).astype(f32)[:, None] * inv[None, :]
    ang_c = (t % 64).astype(f32)[:, None] * inv[None, :]
    ang = np.concatenate([ang_r, ang_r, ang_c, ang_c], axis=1)
    C["rope_cs"] = np.ascontiguousarray(np.stack([np.cos(ang).T, np.sin(ang).T]).astype(f32))
    def pm(n):
        m = np.zeros((n, n), f32)
        for d in range(n):
            if d % 64 < 32:
                m[d + 32, d] = -1
            else:
                m[d - 32, d] = 1
        return m.astype(BF)
    C["PmT"] = pm(128); C["Pm64T"] = pm(64)
    inv2 = (f32(10000.0) ** (-np.linspace(0.0, 1.0, 32, dtype=f32))).astype(f32)
    a2 = np.arange(NL, dtype=f32)[:, None] * inv2[None, :]
    cosl, sinl = np.cos(a2).astype(f32), np.sin(a2).astype(f32)
    cosf = np.concatenate([cosl, np.ones((256, 32), f32)], 0); sinf = np.concatenate([sinl, np.zeros((256, 32), f32)], 0)
    C["rrope"] = np.ascontiguousarray(np.stack([np.concatenate([cosf, cosf], 1).T, np.concatenate([sinf, sinf], 1).T]))
    C["rropet"] = np.ascontiguousarray(np.stack([tiles_tok(cosf, NKT), tiles_tok(sinf, NKT)]))
    m = np.arange(128)[:, None]; c = np.arange(128)[None, :]
    C["rtab"] = np.ascontiguousarray(np.stack([np.maximum(c - m, 0), (c >= m), np.maximum(m - c, 0), (m >= c)], axis=1).astype(f32))
    p = np.arange(128)
    C["rpcol"] = np.stack([127 - p, p], 1).astype(f32)
    cc = np.arange(128)
    C["rqd"] = np.ascontiguousarray(np.broadcast_to(np.stack([cc + 1, 128 - cc])[None], (64, 2, 128)).astype(f32))
    C["ident"] = np.eye(128, dtype=f32)
    C["J"] = np.eye(128, dtype=f32)[::-1].astype(BF)
    def feat(L):
        x = np.arange(2 * L)
        s = np.abs(x - L).astype(f32)
        tn = s / f32(max(L - 1, 1))
        w = f32(2.0 * math.pi) * s / f32(L)
        f = np.linspace(1e-4, 15, 16, dtype=f32)
        return np.ascontiguousarray(np.concatenate([tn[None], np.cos(w[None] * f[:, None]), -np.sin(w[None] * f[:, None])], 0).astype(f32))
    C["feat"] = feat(NL); C["featc"] = feat(256)
    C["deltas"] = np.abs(np.linspace(math.log(1e-2) / 1.5, math.log(1e-2) / 0.3, 512, dtype=f32))
    return C


def na_bias(rpb_h):
    out = np.full((9, 128, 4, 64), -30000.0, np.float32)
    qc = np.arange(64)
    c0 = np.clip(qc - 8, 0, 48)
    for cls in range(9):
        r = cls if cls < 4 else (100 if cls == 4 else 247 + cls)
        r0 = min(max(r - 4, 0), 248)
        for i in range(4):
            for p in range(128):
                kk = i * 128 + p
                w, kc = kk // 64, kk % 64
                ok = (kc >= c0) & (kc < c0 + 16)
                ci = np.clip(kc - qc + 15, 0, 30)
                vals = rpb_h[r0 + w - r + 7, ci]
                out[cls, p, i, :] = np.where(ok, vals, -30000.0)
    return out


def make_mapB(inp, layer, k, PL, PC, PF, C):
    f32 = np.float32
    cw, cb = inp["hy_conv_w"][layer], inp["hy_conv_b"][layer]
    w3 = inp["hy_ffn_w3"][layer]
    b, h = k // 4, k % 4
    F = PF[b]
    m = {}
    m["gq"] = np.ascontiguousarray(F[4608 + 128 * h:4608 + 128 * h + 128])
    m["gk"] = np.ascontiguousarray(F[5120 + 128 * (h // 2):5120 + 128 * (h // 2) + 128])
    m["gv"] = tiles_tok(F[5376 + 128 * (h // 2):5376 + 128 * (h // 2) + 128].T, NKT)
    m["rope_cs"] = C["rope_cs"]; m["PmT"] = C["PmT"]
    m["gnw"] = np.ascontiguousarray(np.stack([inp["gqa_q_norm_w"][layer], inp["gqa_k_norm_w"][layer]], 1).astype(f32))
    m["nq"] = np.ascontiguousarray(F[1536 + 128 * h:1536 + 128 * h + 128])
    m["nk"] = np.ascontiguousarray(F[2048 + 128 * h:2048 + 128 * h + 128])
    V = F[2560 + 128 * h:2560 + 128 * h + 128].T
    m["nve"] = tiles_tok(V, NKT); m["nvo"] = tiles_tok(V[64:], 127)
    m["nbias"] = na_bias(inp["na_rpb"][layer][h])
    m["rq"] = np.ascontiguousarray(F[3072 + 64 * h:3072 + 64 * h + 64])
    m["rk"] = np.ascontiguousarray(F[3328 + 64 * h:3328 + 64 * h + 64])
    m["rkt"] = tiles_tok(F[3328 + 64 * h:3328 + 64 * h + 64].T, NKT)
    m["rvt"] = tiles_tok(F[3584 + 128 * h:3584 + 128 * h + 128].T, NKT)
    m["rg"] = np.ascontiguousarray(F[4096 + 128 * h:4096 + 128 * h + 128])
    m["rrope"] = C["rrope"]; m["rropet"] = C["rropet"]; m["Pm64T"] = C["Pm64T"]
    m["rld"] = np.ascontiguousarray(np.broadcast_to(inp["ret_log_decay"][layer][:, h][None, :], (128, 2)).astype(f32))
    m["rtab"] = C["rtab"]; m["rpcol"] = C["rpcol"]; m["rqd"] = C["rqd"]
    m["rgnw"] = np.ascontiguousarray(inp["ret_gn_w"][layer][128 * h:128 * h + 128].reshape(128, 1).astype(f32))
    def zlay(arr):
        L_ = arr.shape[1]
        z = np.zeros((2, 1), arr.dtype)
        prev = np.concatenate([z, arr[:, :-1]], 1); nxt = np.concatenate([arr[:, 1:], z], 1)
        return [np.ascontiguousarray(a_.reshape(2, L_ // 128, 128).transpose(2, 0, 1).reshape(128, -1)) for a_ in (prev, arr, nxt)]
    def hzbuild(srcs):
        out = []
        for c in range(64):
            row = 64 * k + c
            vs = zlay(np.stack([s_[row] for s_ in srcs]))
            x1s = zlay(np.stack([s_[512 + row] for s_ in srcs]))
            x2s = zlay(np.stack([s_[1024 + row] for s_ in srcs]))
            vf = [np.ascontiguousarray(a_[::-1]) for a_ in vs]
            out.append(np.stack(vs + vf + x1s + x2s, axis=1))
        return np.ascontiguousarray(np.stack(out))
    m["hz"] = hzbuild(PL); m["hzc"] = hzbuild(PC)
    ch = 64 * k + np.arange(64)
    hw = np.stack([np.stack([cw[0, s * 512 + ch], cw[1, s * 512 + ch], cw[2, s * 512 + ch], cb[s * 512 + ch]], -1) for s in range(3)], 1)
    m["hw"] = np.ascontiguousarray(np.broadcast_to(hw[None], (128, 64, 3, 4)).astype(f32))
    m["hb"] = np.ascontiguousarray(np.broadcast_to(inp["hy_bias"][layer][:, ch].T[None], (128, 64, 2)).astype(f32))
    m["fw1"] = inp["hy_ffn_w1"][layer]; m["fw2"] = inp["hy_ffn_w2"][layer]
    m["fw3"] = np.ascontiguousarray(np.stack([np.concatenate([w3[:, o * 1024 + dr * 512 + ch] for o in range(2)], 1) for dr in range(2)], 1))
    m["fsc"] = np.ascontiguousarray(np.stack([inp["hy_ffn_b1"][layer], inp["hy_sin_freq"][layer][0], inp["hy_ffn_b2"][layer], inp["hy_sin_freq"][layer][1]], 1).astype(f32))
    m["ndl"] = np.ascontiguousarray(-np.concatenate([C["deltas"][ch], C["deltas"][ch]]).reshape(128, 1).astype(f32))
    m["feat"] = C["feat"]; m["featc"] = C["featc"]; m["ident"] = C["ident"]; m["J"] = C["J"]
    return m

def kernel(**inp):
    inp = {k: np.asarray(v) for k, v in inp.items()}
    f32 = np.float32
    C = consts()
    x, ctx = inp["x"], inp["ctx"]
    XT = []
    for k in range(8):
        b, q = k // 4, k % 4
        xs = np.concatenate([x[b, q * 4096:(q + 1) * 4096], ctx[b, q * 64:(q + 1) * 64]], axis=0)
        XT.append(np.ascontiguousarray(xs.T.reshape(16, 128, NTOK)))
    cc = [np.ascontiguousarray(np.stack([inp["c"][b].reshape(16, 128).T, inp["c_ctx"].reshape(16, 128).T], axis=-1)) for b in range(2)]
    fo = None
    for layer in range(2):
        aw, ab = inp["ada_w"][layer], inp["ada_b"][layer]
        wA = {"adaw": wlay(aw[:, 0:2 * D]), "adab": vlay(ab[0:2 * D]), "n1w": vlay(inp["norm1_w"][layer]),
              "win": wlay(inp["w_in"][layer][:, :PCOLS])}
        rA = _run("A", [dict(wA, xT=XT[k], cc=cc[k // 4]) for k in range(8)])
        PL, PC = [], []
        for b in range(2):
            ps = [np.asarray(rA[4 * b + q]["pT"]).reshape(PCOLS, NTOK) for q in range(4)]
            PL.append(np.concatenate([p_[:, :4096] for p_ in ps], axis=1))
            PC.append(np.concatenate([p_[:, 4096:] for p_ in ps], axis=1))
        PF = [np.concatenate([PL[b], PC[b]], axis=1) for b in range(2)]
        mapsB = [make_mapB(inp, layer, k, PL, PC, PF, C) for k in range(8)]
        rB = _run("B", mapsB)
        Y = [np.zeros((2048, NL + 256), BF) for _ in range(2)]
        for k in range(8):
            b, h = k // 4, k % 4
            Y[b][512 + 128 * h:512 + 128 * h + 128] = np.asarray(rB[k]["yn"])
            Y[b][1024 + 128 * h:1024 + 128 * h + 128] = np.asarray(rB[k]["yr"])
            Y[b][1536 + 128 * h:1536 + 128 * h + 128] = np.asarray(rB[k]["yg"])
            yh = np.asarray(rB[k]["yh"]).reshape(64, 128, 2, 128)
            yhc = np.asarray(rB[k]["yhc"]).reshape(64, 128, 2, 2)
            for b2 in range(2):
                Y[b2][64 * k:64 * k + 64, :NL] = yh[:, :, b2, :].transpose(0, 2, 1).reshape(64, NL)
                Y[b2][64 * k:64 * k + 64, NL:] = yhc[:, :, b2, :].transpose(0, 2, 1).reshape(64, 256)
        wC = {"adaw": wlay(aw), "adab": vlay(ab), "n1w": vlay(inp["norm1_w"][layer]), "n2w": vlay(inp["norm2_w"][layer]),
              "fnw": vlay(inp["final_norm_w"]), "wg": wlay(inp["w_in"][layer][:, PCOLS:]),
              "wbr": np.concatenate([wlay(inp["w_branch"][layer][i]) for i in range(4)], 0),
              "wo": wlay(inp["w_out"][layer]), "w13": wlay(inp["ffn_w13"][layer]), "w2": wlay(inp["ffn_w2"][layer])}
        mapsC = []
        for k in range(8):
            b, q = k // 4, k % 4
            yt = np.concatenate([Y[b][:, q * 4096:(q + 1) * 4096], Y[b][:, NL + q * 64:NL + (q + 1) * 64]], axis=1)
            mapsC.append(dict(wC, xT=XT[k], yT=np.ascontiguousarray(yt.reshape(16, 128, NTOK)), cc=cc[k // 4]))
        rC = _run("C", mapsC)
        XT = [np.asarray(rC[k]["xo"]) for k in range(8)]
        fo = [np.asarray(rC[k]["fo"]) for k in range(8)]
    out = np.zeros((2, NL, D), np.float32)
    for k in range(8):
        b, q = k // 4, k % 4
        out[b, q * 4096:(q + 1) * 4096] = fo[k].reshape(D, NTOK)[:, :4096].T
    return out
```

```python
import math
import contextlib
import numpy as np
import concourse.bass as bass
import concourse.mybir as mybir
from concourse.bass_utils import run_bass_kernel_spmd

F32 = mybir.dt.float32
BF16 = mybir.dt.bfloat16
ALU = mybir.AluOpType
AF = mybir.ActivationFunctionType
AX = mybir.AxisListType


class Buf:
    def __init__(self, t, name, multi=False):
        self.t = t
        self.name = name
        self.multi = multi
        self.w = {}
        self.r = {}

    def __getitem__(self, idx):
        return View(self, self.t[idx])

    def v(self, ap):
        return View(self, ap)


class View:
    def __init__(self, buf, ap):
        self.buf = buf
        self.ap = ap

    def __getitem__(self, idx):
        return View(self.buf, self.ap[idx])

    def re(self, pat, **kw):
        return View(self.buf, self.ap.rearrange(pat, **kw))


class Op:
    __slots__ = ("eng", "fn", "deps", "needed", "val", "slot", "is_dma", "idx", "key")

    def __init__(self, eng, fn, is_dma):
        self.eng = eng
        self.fn = fn
        self.deps = []
        self.needed = False
        self.val = None
        self.slot = None
        self.is_dma = is_dma


def _ap(x):
    return x.ap if isinstance(x, View) else x


class Prog:
    NSLOT = 6

    def __init__(self, nc, ctx):
        self.nc = nc
        self.ctx = ctx
        self.top = ctx
        self.E = {"pe": nc.tensor, "act": nc.scalar, "dve": nc.vector, "pool": nc.gpsimd, "sp": nc.sync}
        self.ops = {k: [] for k in self.E}
        self.nbuf = 0
        self.ndma = {}

    def sb(self, shape, dt, name=None, multi=False):
        self.nbuf += 1
        name = f"s{self.nbuf}_" + (name or "")
        t = self.ctx.enter_context(self.nc.sbuf_tensor(name, list(shape), dt))
        return Buf(t, name, multi)

    def ps(self, shape, dt, name=None):
        self.nbuf += 1
        name = f"p{self.nbuf}_" + (name or "")
        t = self.ctx.enter_context(self.nc.psum_tensor(name, list(shape), dt))
        return Buf(t, name)

    def dram(self, name, shape, dt, kind="Internal"):
        t = self.nc.dram_tensor(name, list(shape), dt, kind=kind)
        return Buf(t.ap(), name, multi=True)

    def op(self, eng, fn, reads=(), writes=(), is_dma=False):
        o = Op(eng, fn, is_dma)
        if is_dma:
            n = self.ndma.get(eng, 0)
            self.ndma[eng] = n + 1
            o.slot = (eng, n % self.NSLOT)
            o.key = o.slot
        else:
            o.key = eng
        deps = []
        for b in reads:
            deps.extend(b.w.values())
        for b in writes:
            if not b.multi:
                deps.extend(b.w.values())
            deps.extend(b.r.values())
        seen = set()
        for d in deps:
            if id(d) in seen or d is o:
                continue
            seen.add(id(d))
            if d.eng == eng and eng == "pe" and not d.is_dma and not is_dma:
                continue
            o.deps.append(d)
            d.needed = True
        for b in reads:
            b.r[o.key] = o
        for b in writes:
            if b.multi:
                b.w[o.key] = o
            else:
                b.w = {o.key: o}
            b.r = {}
        self.ops[eng].append(o)
        return o

    @staticmethod
    def _bufs(*xs):
        return [x.buf for x in xs if isinstance(x, View)]

    def dma(self, out, in_, eng="sp", **kw):
        o_, i_ = _ap(out), _ap(in_)
        return self.op(eng, lambda e: e.dma_start(out=o_, in_=i_, **kw), self._bufs(in_), self._bufs(out), is_dma=True)

    def mm(self, out, lhsT, rhs, start=True, stop=True, **kw):
        o_, l_, r_ = _ap(out), _ap(lhsT), _ap(rhs)
        return self.op("pe", lambda e: e.matmul(o_, l_, r_, start=start, stop=stop, **kw), self._bufs(lhsT, rhs), self._bufs(out))

    def tr(self, out, in_, ident):
        o_, i_, d_ = _ap(out), _ap(in_), _ap(ident)
        return self.op("pe", lambda e: e.transpose(o_, i_, d_), self._bufs(in_, ident), self._bufs(out))

    def act(self, out, in_, func, bias=None, scale=None, accum_out=None):
        o_, i_ = _ap(out), _ap(in_)
        kw = {}
        rd = self._bufs(in_)
        wr = self._bufs(out)
        if bias is not None:
            kw["bias"] = _ap(bias)
            rd += self._bufs(bias)
        if scale is not None:
            kw["scale"] = _ap(scale)
            rd += self._bufs(scale)
        if accum_out is not None:
            kw["accum_out"] = _ap(accum_out)
            wr += self._bufs(accum_out)
        return self.op("act", lambda e: e.activation(o_, i_, func, **kw), rd, wr)

    def ts(self, eng, out, in0, s1, s2, op0, op1=None, accum_out=None):
        o_, i_ = _ap(out), _ap(in0)
        a1, a2 = _ap(s1), _ap(s2)
        kw = {}
        wr = self._bufs(out)
        if op1 is not None:
            kw["op1"] = op1
        if accum_out is not None:
            kw["accum_out"] = _ap(accum_out)
            wr += self._bufs(accum_out)
        return self.op(eng, lambda e: e.tensor_scalar(o_, i_, a1, a2, op0, **kw), self._bufs(in0, s1, s2), wr)

    def tt(self, eng, out, in0, in1, op):
        o_, a_, b_ = _ap(out), _ap(in0), _ap(in1)
        return self.op(eng, lambda e: e.tensor_tensor(o_, a_, b_, op), self._bufs(in0, in1), self._bufs(out))

    def stt(self, out, in0, scalar, in1, op0, op1, accum_out=None, eng="dve"):
        o_, a_, s_, b_ = _ap(out), _ap(in0), _ap(scalar), _ap(in1)
        kw = {}
        wr = self._bufs(out)
        if accum_out is not None:
            kw["accum_out"] = _ap(accum_out)
            wr += self._bufs(accum_out)
        return self.op(eng, lambda e: e.scalar_tensor_tensor(o_, a_, s_, b_, op0, op1, **kw), self._bufs(in0, scalar, in1), wr)

    def copy(self, eng, out, in_):
        o_, i_ = _ap(out), _ap(in_)
        if eng == "act":
            return self.op(eng, lambda e: e.copy(o_, i_), self._bufs(in_), self._bufs(out))
        return self.op(eng, lambda e: e.tensor_copy(o_, i_), self._bufs(in_), self._bufs(out))

    def memset(self, eng, out, val):
        o_ = _ap(out)
        return self.op(eng, lambda e: e.memset(o_, val), [], self._bufs(out))

    def reduce(self, out, in_, op, axis=AX.X, eng="dve"):
        o_, i_ = _ap(out), _ap(in_)
        return self.op(eng, lambda e: e.tensor_reduce(o_, i_, axis, op), self._bufs(in_), self._bufs(out))

    def recip(self, out, in_, eng="dve"):
        o_, i_ = _ap(out), _ap(in_)
        return self.op(eng, lambda e: e.reciprocal(o_, i_), self._bufs(in_), self._bufs(out))

    def _init_sems(self):
        if hasattr(self, "sems"):
            return
        nc = self.nc
        self.sems = {k: self.top.enter_context(nc.semaphore(f"s_{k}")) for k in self.E}
        self.dsems = {k: [self.top.enter_context(nc.semaphore(f"d_{k}{i}")) for i in range(self.NSLOT)] for k in ("sp", "pool")}
        self.cnt = {k: 0 for k in self.E}
        self.slotcnt = {k: [0] * self.NSLOT for k in ("sp", "pool")}

    def flush(self, final_bufs=()):
        nc = self.nc
        self._init_sems()
        sems, dsems = self.sems, self.dsems
        fin_deps = []
        for b in final_bufs:
            fin_deps.extend(b.w.values())
        for d in fin_deps:
            d.needed = True
        for k in self.E:
            if self.ops[k]:
                last = [o for o in self.ops[k] if not o.is_dma]
                if last:
                    last[-1].needed = True
        for k in self.E:
            for o in self.ops[k]:
                if o.val is not None:
                    continue
                if o.is_dma:
                    s = o.slot[1]
                    self.slotcnt[k][s] += 16
                    o.val = self.slotcnt[k][s]
                elif o.needed:
                    self.cnt[k] += 1
                    o.val = self.cnt[k]
        fin_c = dict(self.cnt)
        fin_d = {k: list(v) for k, v in self.slotcnt.items()}

        def semof(o):
            if o.is_dma:
                return ("d", o.slot), dsems[o.slot[0]][o.slot[1]]
            return ("c", o.eng), sems[o.eng]

        def make(k):
            def body(e):
                seen = {}

                def wait(o):
                    if o.val is None:
                        return
                    key, s = semof(o)
                    if seen.get(key, 0) >= o.val:
                        return
                    seen[key] = o.val
                    e.wait_ge(s, o.val)

                for o in self.ops[k]:
                    for d in o.deps:
                        wait(d)
                    if o.is_dma:
                        key, s = semof(o)
                        if o.val > 16 and seen.get(key, 0) < o.val - 16:
                            seen[key] = o.val - 16
                            e.wait_ge(s, o.val - 16)
                        o.fn(e).then_inc(s, 16)
                    else:
                        ins = o.fn(e)
                        if o.needed:
                            ins.then_inc(sems[k], 1)
                for k2 in self.E:
                    if fin_c[k2] > 0 and seen.get(("c", k2), 0) < fin_c[k2]:
                        e.wait_ge(sems[k2], fin_c[k2])
                for q in fin_d:
                    for i, v in enumerate(fin_d[q]):
                        if v > 0 and seen.get(("d", (q, i)), 0) < v:
                            e.wait_ge(dsems[q][i], v)

            return body

        with nc.Block() as block:
            deco = {"pe": block.tensor, "act": block.scalar, "dve": block.vector, "pool": block.gpsimd, "sp": block.sync}
            for k in self.E:
                deco[k](make(k))
        self.ops = {k: [] for k in self.E}

    def emit(self, final_bufs=()):
        self.flush(final_bufs)

    @contextlib.contextmanager
    def scope(self):
        old = self.ctx
        with contextlib.ExitStack() as st:
            self.ctx = st
            yield
            self.flush()
        self.ctx = old


D = 2048
NKC = 16
EPS = 1e-6


class Pool_:
    def __init__(self, P, n, shape, dt, kind="sb", name="pool", multi=False):
        if kind == "sb":
            self.bufs = [P.sb(shape, dt, f"{name}{i}", multi) for i in range(n)]
        else:
            self.bufs = [P.ps(shape, dt, f"{name}{i}") for i in range(n)]
        self.i = 0

    def get(self):
        b = self.bufs[self.i % len(self.bufs)]
        self.i += 1
        return b


def dview(buf, offset, dims):
    return buf.v(bass.AP(tensor=buf.t.tensor, offset=offset, ap=[list(d) for d in dims]))


def precast(P, srcs):
    outs = []
    with P.scope():
        fp = Pool_(P, 3, [128, 44, 128], F32, "sb", "pcf")
        bp = Pool_(P, 3, [128, 44, 128], BF16, "sb", "pcb")
        i = 0
        for src, name, ncc, nkc in srcs:
            dst = P.dram(name, [ncc, 128, nkc, 128], BF16)
            outs.append(dst)
            for cc in range(ncc):
                a = fp.get(); b = bp.get()
                P.dma(a[:, 0:nkc, :], src[cc], eng="sp")
                eng = ("pool", "dve", "act")[i % 3]
                i += 1
                P.copy(eng, b[:, 0:nkc, :], a[:, 0:nkc, :])
                P.dma(dst[cc], b[:, 0:nkc, :], eng="pool")
    return outs


def linear(P, wpool, wsrc, cc, nkc, rhs_fn, n, pspool, weng="sp"):
    wb = wpool.get()
    P.dma(wb[:, 0:nkc, :], wsrc[cc], eng=weng)
    ps = pspool.get()
    for n0 in range(0, n, 512):
        n1 = min(n, n0 + 512)
        for kc in range(nkc):
            P.mm(ps[:, n0:n1], wb[:, kc, :], rhs_fn(kc)[:, n0:n1], start=(kc == 0), stop=(kc == nkc - 1))
    return ps


def rmsnorm_mod(P, S, xt, hT, n, A, B, j, out_fn=None, post=None):
    sq = S["sq"]
    for kc in range(NKC):
        P.act(sq[:, kc, 0:n], xt[:, kc, 0:n], AF.Square)
    ps = S["ps1"].get()
    for kc in range(NKC):
        P.mm(ps[:, 0:n], S["ones_bf"][:, :], sq[:, kc, 0:n], start=(kc == 0), stop=(kc == NKC - 1))
    rs = S["rstd"]
    P.act(rs[:, 0:n], ps[:, 0:n], AF.Sqrt, bias=S["epsb"][:, 0:1], scale=1.0 / D)
    P.recip(rs[:, 0:n], rs[:, 0:n])
    tmp = S["tmp"]
    for kc in range(NKC):
        P.tt("dve", tmp[:, 0:n], xt[:, kc, 0:n], rs[:, 0:n], ALU.mult)
        o = hT[:, kc, 0:n] if out_fn is None else out_fn(kc)
        P.ts("dve", o, tmp[:, 0:n], A[:, kc, j:j + 1], B[:, kc, j:j + 1], ALU.mult, ALU.add)
        if post is not None:
            post(kc, o)


def mod_vectors(P, S, cc_d, adaw_d, adab_d, nvec):
    cs = P.sb([128, NKC, 2], F32, "cs_s")
    P.dma(cs[:, :, :], cc_d[:, :, :])
    sg = P.sb([128, NKC, 2], F32, "csg")
    P.act(sg[:, :, :], cs[:, :, :], AF.Sigmoid)
    P.tt("dve", cs[:, :, :], cs[:, :, :], sg[:, :, :], ALU.mult)
    ab = P.sb([128, nvec * NKC], F32, "adab_s")
    P.dma(ab[:, :], adab_d[:, :])
    mod = P.sb([128, nvec * NKC, 2], F32, "mod_s")
    wp = S["wpool_f32"]
    for c in range(nvec * NKC):
        wb = wp.get()
        P.dma(wb[:, :, :], adaw_d[c], eng="sp")
        ps = S["ps1"].get()
        for kc in range(NKC):
            P.mm(ps[:, 0:2], wb[:, kc, :], cs[:, kc, :], start=(kc == 0), stop=(kc == NKC - 1))
        P.ts("dve", mod[:, c, :], ps[:, 0:2], ab[:, c:c + 1], None, ALU.add)
    return mod


def common_state(P, nmax):
    S = {}
    S["ones_bf"] = P.sb([128, 128], BF16, "ones_bf")
    P.memset("dve", S["ones_bf"][:, :], 1.0)
    S["epsb"] = P.sb([128, 1], F32, "epsb")
    P.memset("dve", S["epsb"][:, :], EPS)
    S["sq"] = P.sb([128, NKC, nmax], BF16, "sq", multi=True)
    S["rstd"] = P.sb([128, nmax], F32, "rstd")
    S["tmp"] = P.sb([128, nmax], F32, "tmpn")
    S["ps1"] = Pool_(P, 2, [128, 512], F32, "ps", "ps1_")
    S["wpool_f32"] = Pool_(P, 2, [128, NKC, 128], F32, "sb", "wf32_")
    return S

NTOK = 4160
PCOLS = 5632
BLKS = [(i * 512, 512, 0) for i in range(8)] + [(4096, 64, 1)]


def ext(nc, name, shape, dt, out=False):
    return Buf(nc.dram_tensor(name, list(shape), dt, kind="ExternalOutput" if out else "ExternalInput").ap(), name, multi=True)


def load_block(P, xt, xT_d, t0, n):
    for kc in range(NKC):
        P.dma(xt[:, kc, 0:n], xT_d[kc, :, t0:t0 + n], eng="sp")


def build_A():
    nc = bass.Bass("TRN2", target_bir_lowering=False)
    ctx = contextlib.ExitStack()
    with ctx:
        P = Prog(nc, ctx)
        xT_d = ext(nc, "xT", [NKC, 128, NTOK], F32)
        cc_d = ext(nc, "cc", [128, NKC, 2], F32)
        adaw_d = ext(nc, "adaw", [2 * NKC, 128, NKC, 128], F32)
        adab_d = ext(nc, "adab", [128, 2 * NKC], F32)
        nw_d = ext(nc, "n1w", [128, NKC], F32)
        win_d = ext(nc, "win", [PCOLS // 128, 128, NKC, 128], F32)
        pT_d = ext(nc, "pT", [PCOLS // 128, 128, NTOK], BF16, out=True)
        S = common_state(P, 512)
        (win_d,) = precast(P, [(win_d, "win_bf", PCOLS // 128, NKC)])
        mod = mod_vectors(P, S, cc_d, adaw_d, adab_d, 2)
        nw = P.sb([128, NKC], F32, "nw_s")
        P.dma(nw[:, :], nw_d[:, :])
        A1 = P.sb([128, NKC, 2], F32, "A1")
        for j in range(2):
            P.ts("dve", A1[:, :, j], mod[:, NKC:2 * NKC, j], 1.0, None, ALU.add)
            P.tt("dve", A1[:, :, j], A1[:, :, j], nw[:, :], ALU.mult)
        xpool = Pool_(P, 2, [128, NKC, 512], F32, "sb", "xt", multi=True)
        hpool = Pool_(P, 2, [128, NKC, 512], BF16, "sb", "hT", multi=True)
        wpool = Pool_(P, 4, [128, NKC, 128], BF16, "sb", "wb")
        pspool = Pool_(P, 4, [128, 512], F32, "ps", "psl")
        opool = Pool_(P, 3, [128, 512], BF16, "sb", "ost")
        for (t0, n, j) in BLKS:
            xt = xpool.get()
            load_block(P, xt, xT_d, t0, n)
            hT = hpool.get()
            rmsnorm_mod(P, S, xt, hT, n, A1, mod, j)
            for cc in range(PCOLS // 128):
                ps = linear(P, wpool, win_d, cc, NKC, lambda kc: hT[:, kc, :], n, pspool)
                ot = opool.get()
                P.act(ot[:, 0:n], ps[:, 0:n], AF.Copy)
                P.dma(pT_d[cc, :, t0:t0 + n], ot[:, 0:n], eng="sp")
        P.emit(final_bufs=[pT_d])
    return nc


FFH = 5632
NHC = FFH // 128


def build_C():
    nc = bass.Bass("TRN2", target_bir_lowering=False)
    ctx = contextlib.ExitStack()
    with ctx:
        P = Prog(nc, ctx)
        xT_d = ext(nc, "xT", [NKC, 128, NTOK], F32)
        yT_d = ext(nc, "yT", [NKC, 128, NTOK], BF16)
        cc_d = ext(nc, "cc", [128, NKC, 2], F32)
        adaw_d = ext(nc, "adaw", [6 * NKC, 128, NKC, 128], F32)
        adab_d = ext(nc, "adab", [128, 6 * NKC], F32)
        n1_d = ext(nc, "n1w", [128, NKC], F32)
        n2_d = ext(nc, "n2w", [128, NKC], F32)
        fn_d = ext(nc, "fnw", [128, NKC], F32)
        wg_d = ext(nc, "wg", [4 * NKC, 128, NKC, 128], F32)
        wbr_d = ext(nc, "wbr", [4 * NKC, 128, 4, 128], F32)
        wo_d = ext(nc, "wo", [NKC, 128, NKC, 128], F32)
        w13_d = ext(nc, "w13", [2 * NHC, 128, NKC, 128], F32)
        w2_d = ext(nc, "w2", [NKC, 128, NHC, 128], F32)
        xo_d = ext(nc, "xo", [NKC, 128, NTOK], F32, out=True)
        fo_d = ext(nc, "fo", [NKC, 128, NTOK], F32, out=True)
        S = common_state(P, 512)
        wg_d, wbr_d, wo_d, w13_d, w2_d = precast(P, [(wg_d, "wg_bf", 4 * NKC, NKC), (wbr_d, "wbr_bf", 4 * NKC, 4), (wo_d, "wo_bf", NKC, NKC),
                                                      (w13_d, "w13_bf", 2 * NHC, NKC), (w2_d, "w2_bf", NKC, NHC)])
        mod = mod_vectors(P, S, cc_d, adaw_d, adab_d, 6)
        nws = P.sb([128, 3, NKC], F32, "nws")
        P.dma(nws[:, 0, :], n1_d[:, :]); P.dma(nws[:, 1, :], n2_d[:, :]); P.dma(nws[:, 2, :], fn_d[:, :])
        A1 = P.sb([128, NKC, 2], F32, "A1")
        A2 = P.sb([128, NKC, 2], F32, "A2")
        A3 = P.sb([128, NKC, 2], F32, "A3")
        Z3 = P.sb([128, NKC, 2], F32, "Z3")
        P.memset("dve", Z3[:, :, :], 0.0)
        for j in range(2):
            P.ts("dve", A1[:, :, j], mod[:, NKC:2 * NKC, j], 1.0, None, ALU.add)
            P.tt("dve", A1[:, :, j], A1[:, :, j], nws[:, 0, :], ALU.mult)
            P.ts("dve", A2[:, :, j], mod[:, 4 * NKC:5 * NKC, j], 1.0, None, ALU.add)
            P.tt("dve", A2[:, :, j], A2[:, :, j], nws[:, 1, :], ALU.mult)
            P.copy("dve", A3[:, :, j], nws[:, 2, :])
        xpool = Pool_(P, 1, [128, NKC, 512], F32, "sb", "xt", multi=True)
        hT = P.sb([128, NKC, 512], BF16, "hT", multi=True)
        yT = P.sb([128, NKC, 512], BF16, "yT", multi=True)
        mT = P.sb([128, NKC, 512], BF16, "mT", multi=True)
        uT = P.sb([128, NHC, 512], BF16, "uT", multi=True)
        fpool = Pool_(P, 2, [128, 512], F32, "sb", "fst")
        wpool = Pool_(P, 3, [128, NHC, 128], BF16, "sb", "wb")
        pspool = Pool_(P, 4, [128, 512], F32, "ps", "psl")
        sgp = Pool_(P, 2, [128, 512], BF16, "sb", "sg")
        prp = Pool_(P, 2, [128, 512], F32, "sb", "pr")
        acc = P.sb([128, 512], F32, "acc")
        for (t0, n, j) in BLKS:
            xt = xpool.get()
            load_block(P, xt, xT_d, t0, n)
            for c in range(NKC):
                P.dma(yT[:, c, 0:n], yT_d[c, :, t0:t0 + n], eng="sp")
            rmsnorm_mod(P, S, xt, hT, n, A1, mod, j)
            for cc in range(NKC):
                for i in range(4):
                    psg = linear(P, wpool, wg_d, i * NKC + cc, NKC, lambda kc: hT[:, kc, :], n, pspool)
                    sg = sgp.get()
                    P.act(sg[:, 0:n], psg[:, 0:n], AF.Sigmoid)
                    psb = linear(P, wpool, wbr_d, i * NKC + cc, 4, lambda kc: yT[:, i * 4 + kc, :], n, pspool)
                    if i == 0:
                        P.tt("dve", acc[:, 0:n], psb[:, 0:n], sg[:, 0:n], ALU.mult)
                    else:
                        pr = prp.get()
                        P.tt("dve", pr[:, 0:n], psb[:, 0:n], sg[:, 0:n], ALU.mult)
                        if i < 3:
                            P.tt("pool", acc[:, 0:n], acc[:, 0:n], pr[:, 0:n], ALU.add)
                        else:
                            P.tt("pool", mT[:, cc, 0:n], acc[:, 0:n], pr[:, 0:n], ALU.add)
            for cc in range(NKC):
                ps = linear(P, wpool, wo_d, cc, NKC, lambda kc: mT[:, kc, :], n, pspool)
                P.stt(xt[:, cc, 0:n], ps[:, 0:n], mod[:, 2 * NKC + cc, j:j + 1], xt[:, cc, 0:n], ALU.mult, ALU.add)
            rmsnorm_mod(P, S, xt, hT, n, A2, mod[:, 3 * NKC:4 * NKC, :], j)
            for hc in range(NHC):
                psa = linear(P, wpool, w13_d, hc, NKC, lambda kc: hT[:, kc, :], n, pspool)
                sg = sgp.get()
                P.act(sg[:, 0:n], psa[:, 0:n], AF.Silu)
                psb = linear(P, wpool, w13_d, NHC + hc, NKC, lambda kc: hT[:, kc, :], n, pspool)
                P.tt("dve", uT[:, hc, 0:n], psb[:, 0:n], sg[:, 0:n], ALU.mult)
            for cc in range(NKC):
                ps = linear(P, wpool, w2_d, cc, NHC, lambda kc: uT[:, kc, :], n, pspool)
                P.stt(xt[:, cc, 0:n], ps[:, 0:n], mod[:, 5 * NKC + cc, j:j + 1], xt[:, cc, 0:n], ALU.mult, ALU.add)
            for kc in range(NKC):
                P.dma(xo_d[kc, :, t0:t0 + n], xt[:, kc, 0:n], eng="sp")
            rmsnorm_mod(P, S, xt, None, n, A3, Z3, j, out_fn=lambda kc: fpool.get()[:, 0:n],
                        post=lambda kc, o: P.dma(fo_d[kc, :, t0:t0 + n], o, eng="sp"))
        P.emit(final_bufs=[xo_d, fo_d])
    return nc


NL = 16384
NKT = 130
SCALE = 128 ** -0.5
TWO_PI = 6.283185307179586


def norm_rope(P, S, src, n, nw_col, cs_d, t0, PmT, out, rope, sbp):
    sq = sbp["sq"].get()
    P.act(sq[:, 0:n], src, AF.Square)
    ps = S["ps1"].get()
    P.mm(ps[:, 0:n], S["ones_bf"][:, :], sq[:, 0:n])
    rs = sbp["rs"].get()
    P.act(rs[:, 0:n], ps[:, 0:n], AF.Sqrt, bias=S["epsb"][:, 0:1], scale=1.0 / 128)
    P.recip(rs[:, 0:n], rs[:, 0:n])
    if not rope:
        P.stt(out, src, nw_col, rs[:, 0:n], ALU.mult, ALU.mult)
        return
    kn = sbp["kn"].get()
    P.stt(kn[:, 0:n], src, nw_col, rs[:, 0:n], ALU.mult, ALU.mult)
    cs = sbp["cs"].get()
    P.dma(cs[:, 0, 0:n], cs_d[0, :, t0:t0 + n])
    P.dma(cs[:, 1, 0:n], cs_d[1, :, t0:t0 + n])
    ps2 = S["ps1"].get()
    P.mm(ps2[:, 0:n], PmT[:, :], kn[:, 0:n])
    t1 = sbp["t1"].get()
    P.tt("pool", t1[:, 0:n], kn[:, 0:n], cs[:, 0, 0:n], ALU.mult)
    t2 = sbp["t2"].get()
    P.tt("dve", t2[:, 0:n], ps2[:, 0:n], cs[:, 1, 0:n], ALU.mult)
    P.tt("dve", out, t1[:, 0:n], t2[:, 0:n], ALU.add)


def attn_core(P, S, KT, Vt, key_tiles, qr, n, pss, ps_o, ps_l, epool, out_d, o0, opool, bias=None):
    nk = len(key_tiles)
    for i, (kv_, vv_) in enumerate(key_tiles):
        ps = pss.get()
        P.mm(ps[:, 0:n], kv_, qr)
        e = epool.get()
        if bias is not None and bias[i] is not None:
            tb = S["tb"].get()
            P.stt(tb[:, 0:n], ps[:, 0:n], SCALE, bias[i], ALU.mult, ALU.add)
            P.act(e[:, 0:n], tb[:, 0:n], AF.Exp)
        else:
            P.act(e[:, 0:n], ps[:, 0:n], AF.Exp, scale=SCALE)
        P.mm(ps_o[:, 0:n], vv_, e[:, 0:n], start=(i == 0), stop=(i == nk - 1))
        P.mm(ps_l[:, 0:n], S["ones_bf"][:, :], e[:, 0:n], start=(i == 0), stop=(i == nk - 1))
    rl = S["rl"].get()
    P.recip(rl[:, 0:n], ps_l[:, 0:n])
    o = opool.get()
    P.tt("dve", o[:, 0:n], ps_o[:, 0:n], rl[:, 0:n], ALU.mult)
    P.dma(out_d[:, o0:o0 + n], o[:, 0:n])


def gqa_mixer(P, S, nc, D_):
    with P.scope():
        KT = P.sb([128, NL + 256], BF16, "KT", multi=True)
        Vt = P.sb([128, NKT, 128], BF16, "Vt")
        P.dma(Vt[:, 0:65, :], D_["gv"][:, 0:65, :]); P.dma(Vt[:, 65:NKT, :], D_["gv"][:, 65:NKT, :])
        PmT = P.sb([128, 128], BF16, "PmT"); P.dma(PmT[:, :], D_["PmT"][:, :])
        gnw = P.sb([128, 2], F32, "gnw"); P.dma(gnw[:, :], D_["gnw"][:, :])
        sbp = {"sq": Pool_(P, 2, [128, 512], BF16, "sb", "gsq"), "rs": Pool_(P, 2, [128, 512], F32, "sb", "grs"),
               "kn": Pool_(P, 2, [128, 512], BF16, "sb", "gkn"), "cs": Pool_(P, 2, [128, 2, 512], F32, "sb", "gcs", multi=True),
               "t1": Pool_(P, 2, [128, 512], F32, "sb", "gt1"), "t2": Pool_(P, 2, [128, 512], F32, "sb", "gt2")}
        inp = Pool_(P, 2, [128, 512], BF16, "sb", "gin")
        for kb in range(33):
            t0 = kb * 512
            n = 512 if kb < 32 else 256
            x = inp.get()
            P.dma(x[:, 0:n], D_["gk"][:, t0:t0 + n])
            norm_rope(P, S, x[:, 0:n], n, gnw[:, 1:2], D_["rope_cs"], t0, PmT, KT[:, t0:t0 + n], kb < 32, sbp)
        pss = Pool_(P, 3, [128, 512], F32, "ps", "gps")
        ps_o = P.ps([128, 512], F32, "gpo"); ps_l = P.ps([128, 512], F32, "gpl")
        epool = Pool_(P, 3, [128, 512], BF16, "sb", "ge")
        opool = Pool_(P, 2, [128, 512], BF16, "sb", "go")
        qrp = Pool_(P, 2, [128, 512], BF16, "sb", "gqr")
        S["rl"] = Pool_(P, 2, [128, 512], F32, "sb", "grl")
        for qb in range(33):
            t0 = qb * 512
            n = 512 if qb < 32 else 256
            x = inp.get()
            P.dma(x[:, 0:n], D_["gq"][:, t0:t0 + n])
            qr = qrp.get()
            norm_rope(P, S, x[:, 0:n], n, gnw[:, 0:1], D_["rope_cs"], t0, PmT, qr[:, 0:n], qb < 32, sbp)
            tiles = range(NKT) if qb < 32 else range(128, NKT)
            kt = [(KT[:, i * 128:(i + 1) * 128], Vt[:, i, :]) for i in tiles]
            attn_core(P, S, KT, Vt, kt, qr[:, 0:n], n, pss, ps_o, ps_l, epool, D_["yg"], t0, opool)


def na_mixer(P, S, nc, D_):
    with P.scope():
        KT = P.sb([128, NL + 256], BF16, "nKT")
        P.dma(KT[:, 0:8192], D_["nk"][:, 0:8192]); P.dma(KT[:, 8192:NL + 256], D_["nk"][:, 8192:NL + 256])
        Ve = P.sb([128, NKT, 128], BF16, "nVe"); Vo = P.sb([128, 127, 128], BF16, "nVo")
        P.dma(Ve[:, 0:65, :], D_["nve"][:, 0:65, :]); P.dma(Ve[:, 65:NKT, :], D_["nve"][:, 65:NKT, :])
        P.dma(Vo[:, 0:64, :], D_["nvo"][:, 0:64, :]); P.dma(Vo[:, 64:127, :], D_["nvo"][:, 64:127, :])
        nb = P.sb([128, 9, 4, 64], F32, "nbias")
        for c in range(9):
            P.dma(nb[:, c, :, :], D_["nbias"][c])
        pss = Pool_(P, 3, [128, 512], F32, "ps", "nps")
        ps_o = P.ps([128, 512], F32, "npo"); ps_l = P.ps([128, 512], F32, "npl")
        epool = Pool_(P, 3, [128, 512], BF16, "sb", "ne")
        opool = Pool_(P, 2, [128, 512], BF16, "sb", "no")
        S["rl"] = Pool_(P, 2, [128, 512], F32, "sb", "nrl")
        S["tb"] = Pool_(P, 3, [128, 512], F32, "sb", "ntb")
        qp = Pool_(P, 2, [128, 2048], BF16, "sb", "nq")
        for r in range(256):
            if r % 32 == 0:
                qt = qp.get()
                P.dma(qt[:, :], D_["nq"][:, r * 64:r * 64 + 2048])
            qr = qt[:, (r % 32) * 64:(r % 32) * 64 + 64]
            r0 = min(max(r - 4, 0), 248)
            cls = r if r < 4 else (4 if r < 252 else r - 247)
            kt, bs = [], []
            for i in range(4):
                tk = r0 * 64 + i * 128
                vv = Ve[:, tk // 128, :] if r0 % 2 == 0 else Vo[:, (tk - 64) // 128, :]
                kt.append((KT[:, tk:tk + 128], vv)); bs.append(nb[:, cls, i, :])
            for i in (128, 129):
                kt.append((KT[:, i * 128:(i + 1) * 128], Ve[:, i, :])); bs.append(None)
            attn_core(P, S, KT, None, kt, qr, 64, pss, ps_o, ps_l, epool, D_["yn"], r * 64, opool, bias=bs)
        qt = qp.get()
        P.dma(qt[:, 0:256], D_["nq"][:, NL:NL + 256])
        kt = [(KT[:, i * 128:(i + 1) * 128], Ve[:, i, :]) for i in (128, 129)]
        attn_core(P, S, KT, None, kt, qt[:, 0:256], 256, pss, ps_o, ps_l, epool, D_["yn"], NL, opool)


def ret_mixer(P, S, nc, D_):
    NT = NL + 256
    with P.scope():
        tab = P.sb([128, 4, 128], F32, "rtab"); P.dma(tab[:, :, :], D_["rtab"][:, :, :])
        pcol = P.sb([128, 2], F32, "rpcol"); P.dma(pcol[:, :], D_["rpcol"][:, :])
        qdt = P.sb([64, 2, 128], F32, "rqdt"); P.dma(qdt[:, :, :], D_["rqd"][:, :, :])
        ld = P.sb([128, 2], F32, "rld"); P.dma(ld[:, :], D_["rld"][:, :])
        gnw = P.sb([128, 1], F32, "rgnw"); P.dma(gnw[:, :], D_["rgnw"][:, :])
        Pm = P.sb([64, 64], BF16, "rPm"); P.dma(Pm[:, :], D_["Pm64T"][:, :])
        lg = P.sb([128, 2], F32, "rlg")
        P.ts("dve", lg[:, :], ld[:, :], -1.0, None, ALU.mult)
        P.tt("dve", lg[:, :], lg[:, :], ld[:, :], ALU.min)
        ksc = 64 ** -0.5
        intra = P.sb([128, 2, 128], BF16, "rintra", multi=True)
        kdec = P.sb([128, 2], F32, "rkdec", multi=True)
        qdec = P.sb([64, 2, 128], F32, "rqdec", multi=True)
        gC = P.sb([64, 2], F32, "rgC", multi=True)
        e1 = P.sb([128, 128], F32, "re1")
        for dr in range(2):
            P.act(e1[:, :], tab[:, 2 * dr, :], AF.Exp, scale=lg[:, dr:dr + 1])
            P.stt(intra[:, dr, :], e1[:, :], ksc, tab[:, 2 * dr + 1, :], ALU.mult, ALU.mult)
            P.act(kdec[:, dr:dr + 1], pcol[:, dr:dr + 1], AF.Exp, scale=lg[:, dr:dr + 1])
            P.act(qdec[:, dr, :], qdt[:, dr, :], AF.Exp, scale=lg[0:64, dr:dr + 1])
            P.act(gC[:, dr:dr + 1], lg[0:64, dr:dr + 1], AF.Exp, scale=128.0)
        P.ts("dve", kdec[:, :], kdec[:, :], ksc, None, ALU.mult)
        vt = P.sb([128, NKT, 128], BF16, "rvt"); P.dma(vt[:, 0:65, :], D_["rvt"][:, 0:65, :]); P.dma(vt[:, 65:NKT, :], D_["rvt"][:, 65:NKT, :])
        SP = P.sb([64, 2, NKT, 128], BF16, "rSP", multi=True)
        St = P.sb([64, 2, 128], F32, "rS")
        P.memset("dve", St[:, :, :], 0.0)
        order = [[128, 129] + list(range(128)), [129, 128] + list(range(127, -1, -1))]
        with P.scope():
            ktr = P.sb([128, NKT, 64], BF16, "rktr", multi=True)
            with P.scope():
                kt = P.sb([128, NKT, 64], BF16, "rkt"); P.dma(kt[:, :, :], D_["rkt"][:, :, :])
                ct = P.sb([128, 2, NKT, 32], F32, "rct", multi=True)
                P.dma(ct[:, 0, :, :], D_["rropet"][0]); P.dma(ct[:, 1, :, :], D_["rropet"][1])
                ta = P.sb([128, NKT, 32], F32, "rta"); tb_ = P.sb([128, NKT, 32], F32, "rtb")
                x1, x2 = kt[:, :, 0:32], kt[:, :, 32:64]
                P.tt("dve", ta[:, :, :], x1, ct[:, 0, :, :], ALU.mult); P.tt("pool", tb_[:, :, :], x2, ct[:, 1, :, :], ALU.mult)
                P.tt("dve", ktr[:, :, 0:32], ta[:, :, :], tb_[:, :, :], ALU.subtract)
                P.tt("dve", ta[:, :, :], x1, ct[:, 1, :, :], ALU.mult); P.tt("pool", tb_[:, :, :], x2, ct[:, 0, :, :], ALU.mult)
                P.tt("dve", ktr[:, :, 32:64], ta[:, :, :], tb_[:, :, :], ALU.add)
            kd = P.sb([128, NKT, 64], BF16, "rkd")
            KV = P.sb([64, NKT, 128], BF16, "rKV", multi=True)
            pskv = Pool_(P, 2, [128, 512], F32, "ps", "rpk")
            for dr in range(2):
                P.ts("dve", kd[:, :, :], ktr[:, :, :], kdec[:, dr:dr + 1], None, ALU.mult)
                for c0 in range(0, NKT, 4):
                    ps = pskv.get()
                    cn = min(4, NKT - c0)
                    for i in range(cn):
                        P.mm(ps[0:64, i * 128:(i + 1) * 128], kd[:, c0 + i, :], vt[:, c0 + i, :])
                    P.act(KV[:, c0:c0 + cn, :], ps[0:64, 0:cn * 128].re("p (a b) -> p a b", b=128), AF.Copy)
                for step in range(NKT):
                    c = order[dr][step]
                    P.copy("pool", SP[:, dr, c, :], St[:, dr, :])
                    P.stt(St[:, dr, :], St[:, dr, :], gC[:, dr:dr + 1], KV[:, c, :], ALU.mult, ALU.add)
        cs = P.sb([64, 2, 2048], F32, "rcs", multi=True)
        qin = Pool_(P, 2, [64, 2, 2048], BF16, "sb", "rqk", multi=True)
        qr = P.sb([64, 2, 2048], BF16, "rqr", multi=True)
        ta = P.sb([64, 2048], F32, "rta2"); tb_ = P.sb([64, 2048], F32, "rtb2")
        gin = Pool_(P, 2, [128, 512], BF16, "sb", "rgin")
        pss = Pool_(P, 2, [128, 512], F32, "ps", "rps")
        pso = Pool_(P, 2, [128, 512], F32, "ps", "rpo")
        smp = Pool_(P, 4, [128, 128], BF16, "sb", "rsm")
        qdp = Pool_(P, 4, [64, 128], BF16, "sb", "rqd")
        sqp = Pool_(P, 2, [128, 512], BF16, "sb", "rsq"); rsp = Pool_(P, 2, [128, 512], F32, "sb", "rrs")
        sgp = Pool_(P, 2, [128, 512], F32, "sb", "rsg"); t3p = Pool_(P, 2, [128, 512], F32, "sb", "rt3")
        op = Pool_(P, 2, [128, 512], BF16, "sb", "ro")
        for sb_ in range(0, NT, 2048):
            n = min(2048, NT - sb_)
            qk = qin.get()
            P.dma(qk[:, 0, 0:n], D_["rq"][:, sb_:sb_ + n]); P.dma(qk[:, 1, 0:n], D_["rk"][:, sb_:sb_ + n])
            P.dma(cs[:, 0, 0:n], D_["rrope"][0, :, sb_:sb_ + n]); P.dma(cs[:, 1, 0:n], D_["rrope"][1, :, sb_:sb_ + n])
            for w in range(2):
                for n0 in range(0, n, 512):
                    n1 = min(n, n0 + 512)
                    ps = pss.get()
                    P.mm(ps[0:64, 0:n1 - n0], Pm[:, :], qk[:, w, n0:n1])
                    P.tt("dve", tb_[:, n0:n1], ps[0:64, 0:n1 - n0], cs[:, 1, n0:n1], ALU.mult)
                P.tt("pool", ta[:, 0:n], qk[:, w, 0:n], cs[:, 0, 0:n], ALU.mult)
                P.tt("dve", qr[:, w, 0:n], ta[:, 0:n], tb_[:, 0:n], ALU.add)
            for g0 in range(0, n, 512):
                gn = min(512, n - g0)
                po = pso.get()
                gt = gin.get()
                P.dma(gt[:, 0:gn], D_["rg"][:, sb_ + g0:sb_ + g0 + gn])
                for c0 in range(0, gn, 128):
                    ch = (sb_ + g0 + c0) // 128
                    lo = g0 + c0
                    ps = pss.get()
                    P.mm(ps[:, 0:128], qr[:, 1, lo:lo + 128], qr[:, 0, lo:lo + 128])
                    first = True
                    for dr in range(2):
                        sm = smp.get()
                        P.tt("dve", sm[:, :], ps[:, 0:128], intra[:, dr, :], ALU.mult)
                        qd = qdp.get()
                        P.tt("pool", qd[:, :], qr[:, 0, lo:lo + 128], qdec[:, dr, :], ALU.mult)
                        P.mm(po[:, c0:c0 + 128], vt[:, ch, :], sm[:, :], start=first, stop=False)
                        first = False
                        P.mm(po[:, c0:c0 + 128], SP[:, dr, ch, :], qd[:, :], start=False, stop=(dr == 1))
                sq = sqp.get()
                P.act(sq[:, 0:gn], po[:, 0:gn], AF.Square)
                ps = pss.get()
                P.mm(ps[:, 0:gn], S["ones_bf"][:, :], sq[:, 0:gn])
                rs = rsp.get()
                P.act(rs[:, 0:gn], ps[:, 0:gn], AF.Sqrt, bias=S["epsb"][:, 0:1], scale=1.0 / 128)
                P.recip(rs[:, 0:gn], rs[:, 0:gn])
                sg = sgp.get()
                P.act(sg[:, 0:gn], gt[:, 0:gn], AF.Silu)
                t3 = t3p.get()
                P.stt(t3[:, 0:gn], po[:, 0:gn], gnw[:, 0:1], rs[:, 0:gn], ALU.mult, ALU.mult)
                o = op.get()
                P.tt("dve", o[:, 0:gn], t3[:, 0:gn], sg[:, 0:gn], ALU.mult)
                P.dma(D_["yr"][:, sb_ + g0:sb_ + g0 + gn], o[:, 0:gn])


PI = 3.141592653589793


def hyena_filter(P, S, D_, L, feat_d, hf_d, rnB, col0):
    with P.scope():
        w1 = P.sb([33, 64], F32, "fw1"); P.dma(w1[:, :], D_["fw1"][:, :])
        w2 = P.sb([64, 64], F32, "fw2"); P.dma(w2[:, :], D_["fw2"][:, :])
        w3 = P.sb([64, 2, 128], F32, "fw3"); P.dma(w3[:, :, :], D_["fw3"][:, :, :])
        fsc = P.sb([64, 4], F32, "fsc"); P.dma(fsc[:, :], D_["fsc"][:, :])
        ndl = P.sb([128, 1], F32, "ndl"); P.dma(ndl[:, :], D_["ndl"][:, :])
        CS = min(512, L)
        nch = (2 * L) // CS
        ss = P.sb([128, nch], F32, "fss", multi=True)
        ftp = Pool_(P, 2, [33, 512], F32, "sb", "fft")
        tnp = Pool_(P, 2, [128, 512], F32, "sb", "ftn")
        zp = Pool_(P, 4, [64, 512], F32, "sb", "fz")
        mp = Pool_(P, 4, [64, 512], F32, "sb", "fm")
        dcp = Pool_(P, 2, [128, 512], F32, "sb", "fdc")
        hbp = Pool_(P, 2, [128, 512], BF16, "sb", "fhb")
        sqj = P.sb([128, 512], F32, "fsqj")
        psp = Pool_(P, 2, [128, 512], F32, "ps", "fps")
        mpi = P.sb([64, 1], F32, "fmpi"); P.memset("dve", mpi[:, :], 0.0)

        def sin_layer(ps, bcol, fcol):
            z = zp.get()
            P.ts("dve", z[:, 0:CS], ps[0:64, 0:CS], fsc[:, bcol:bcol + 1], fsc[:, fcol:fcol + 1], ALU.add, ALU.mult)
            m1 = mp.get(); m2 = mp.get()
            P.ts("pool", m1[:, 0:CS], z[:, 0:CS], PI, -TWO_PI, ALU.is_gt, ALU.mult)
            P.ts("pool", m2[:, 0:CS], z[:, 0:CS], -PI, TWO_PI, ALU.is_lt, ALU.mult)
            P.tt("dve", m1[:, 0:CS], m1[:, 0:CS], m2[:, 0:CS], ALU.add)
            P.tt("dve", z[:, 0:CS], z[:, 0:CS], m1[:, 0:CS], ALU.add)
            P.act(z[:, 0:CS], z[:, 0:CS], AF.Sin)
            return z

        for ch in range(nch):
            x0 = ch * CS
            ft = ftp.get(); P.dma(ft[:, 0:CS], feat_d[:, x0:x0 + CS])
            tn = tnp.get()
            P.dma(tn[:, 0:CS], dview(feat_d, x0, [[0, 128], [1, CS]]))
            ps = psp.get()
            P.mm(ps[0:64, 0:CS], w1[:, :], ft[:, 0:CS])
            z1 = sin_layer(ps, 0, 1)
            ps = psp.get()
            P.mm(ps[0:64, 0:CS], w2[:, :], z1[:, 0:CS])
            z2 = sin_layer(ps, 2, 3)
            ps = psp.get()
            dirn = 1 if x0 < L else 0
            P.mm(ps[:, 0:CS], w3[:, dirn, :], z2[:, 0:CS])
            dc = dcp.get()
            P.act(dc[:, 0:CS], tn[:, 0:CS], AF.Exp, scale=ndl[:, 0:1])
            hb = hbp.get()
            P.tt("dve", hb[:, 0:CS], ps[:, 0:CS], dc[:, 0:CS], ALU.mult)
            lo = 1 if ch == 0 else 0
            P.act(sqj[:, lo:CS], hb[:, lo:CS], AF.Square, accum_out=ss[:, ch:ch + 1])
            P.dma(hf_d[:, x0:x0 + CS], hb[:, 0:CS])
        tot = P.sb([128, 1], F32, "ftot")
        P.reduce(tot[:, :], ss[:, :], ALU.add)
        P.act(tot[:, :], tot[:, :], AF.Sqrt, bias=S["epsb"][:, 0:1], scale=1.0)
        P.recip(tot[:, :], tot[:, :])
        idt = P.sb([128, 128], F32, "fid"); P.dma(idt[:, :], D_["ident"][:, :])
        dg = P.sb([128, 128], F32, "fdg")
        P.ts("dve", dg[:, :], idt[:, :], tot[:, 0:1], None, ALU.mult)
        onesf = P.sb([128, 128], F32, "fones"); P.memset("dve", onesf[:, :], 1.0)
        ps = psp.get()
        P.mm(ps[:, 0:128], onesf[:, :], dg[:, :])
        P.copy("dve", rnB[:, col0:col0 + 128], ps[:, 0:128])


def hyena_conv(P, S, D_, L, hz_d, hf_d, rnB, col0, yh_d, hw, hbias, J):
    nb = L // 128
    NB2 = 2 * nb
    W = 2 * L - 127
    with P.scope():
        hsp = Pool_(P, 2, [128, W], BF16, "sb", "hHS", multi=True)
        zp = Pool_(P, 2, [128, 12, NB2], BF16, "sb", "hz")
        up = Pool_(P, 2, [128, 4, NB2], BF16, "sb", "hu", multi=True)
        tp = Pool_(P, 4, [128, NB2], F32, "sb", "ht")
        zfp = Pool_(P, 2, [128, NB2], BF16, "sb", "hzf")
        z2p = Pool_(P, 2, [128, NB2], BF16, "sb", "hz2")
        yp = Pool_(P, 2, [128, NB2], BF16, "sb", "hy")
        psc = Pool_(P, 2, [128, 512], F32, "ps", "hpc")
        psj = Pool_(P, 2, [128, 512], F32, "ps", "hpj")
        for c in range(64):
            z = zp.get()
            P.dma(z[:, :, :], hz_d[c])
            u = up.get()
            for s in range(4):
                ws = 0 if s < 2 else s - 1
                t = tp.get()
                P.ts("dve", t[:, :], z[:, 3 * s + 0, :], hw[:, c, ws, 0:1], hw[:, c, ws, 3:4], ALU.mult, ALU.add)
                P.stt(t[:, :], z[:, 3 * s + 1, :], hw[:, c, ws, 1:2], t[:, :], ALU.mult, ALU.add)
                P.stt(u[:, s, :], z[:, 3 * s + 2, :], hw[:, c, ws, 2:3], t[:, :], ALU.mult, ALU.add)
            zin, zfl = u[:, 0, :], u[:, 1, :]
            for o in range(2):
                oc = o * 64 + c
                hs = hsp.get()
                half = W // 2
                P.dma(hs[:, 0:half], dview(hf_d, oc * 2 * L, [[1, 128], [1, half]]))
                P.dma(hs[:, half:W], dview(hf_d, oc * 2 * L + half, [[1, 128], [1, W - half]]))
                ps = psc.get()
                pv = ps[:, 0:NB2].re("p (b a) -> p b a", b=2)
                zv = zfl.re("p (b a) -> p b a", b=2)
                ds = [0] + [d for d in range(-(nb - 1), nb) if d != 0]
                for i, d in enumerate(ds):
                    lo, hi = max(0, d), min(nb - 1, nb - 1 + d)
                    c0 = L + 128 * d - 127
                    P.mm(pv[:, :, lo:hi + 1], hs[:, c0:c0 + 128], zv[:, :, lo - d:hi - d + 1],
                         start=(i == 0), stop=(i == len(ds) - 1), skip_group_check=True)
                t1 = tp.get()
                P.ts("dve", t1[:, :], zin, hbias[:, c, o:o + 1], None, ALU.mult)
                t2 = tp.get()
                P.stt(t2[:, :], ps[:, 0:NB2], rnB[:, col0 + oc:col0 + oc + 1], t1[:, :], ALU.mult, ALU.add)
                if o == 0:
                    z2 = z2p.get()
                    P.tt("dve", z2[:, :], t2[:, :], u[:, 2, :], ALU.mult)
                    pj = psj.get()
                    P.mm(pj[:, 0:NB2], J[:, :], z2[:, :])
                    zf2 = zfp.get()
                    P.copy("act", zf2[:, :], pj[:, 0:NB2])
                    zin, zfl = z2[:, :], zf2[:, :]
                else:
                    y = yp.get()
                    P.tt("dve", y[:, :], t2[:, :], u[:, 3, :], ALU.mult)
                    P.dma(yh_d[c], y[:, :])


def hyena_mixer(P, S, nc, D_):
    with P.scope():
        rnB = P.sb([128, 256], F32, "hrnB", multi=True)
        hw = P.sb([128, 64, 3, 4], F32, "hhw"); P.dma(hw[:, :, :, :], D_["hw"][:, :, :, :])
        hb = P.sb([128, 64, 2], F32, "hhb"); P.dma(hb[:, :, :], D_["hb"][:, :, :])
        J = P.sb([128, 128], BF16, "hJ"); P.dma(J[:, :], D_["J"][:, :])
        hyena_filter(P, S, D_, NL, D_["feat"], D_["hf"], rnB, 0)
        hyena_filter(P, S, D_, 256, D_["featc"], D_["hfc"], rnB, 128)
        hyena_conv(P, S, D_, NL, D_["hz"], D_["hf"], rnB, 0, D_["yh"], hw, hb, J)
        hyena_conv(P, S, D_, 256, D_["hzc"], D_["hfc"], rnB, 128, D_["yhc"], hw, hb, J)


def build_B():
    nc = bass.Bass("TRN2", target_bir_lowering=False)
    ctx = contextlib.ExitStack()
    NT = NL + 256
    with ctx:
        P = Prog(nc, ctx)
        D_ = {}
        def I(name, shape, dt): D_[name] = ext(nc, name, shape, dt)
        def O(name, shape, dt): D_[name] = ext(nc, name, shape, dt, out=True)
        I("gq", [128, NT], BF16); I("gk", [128, NT], BF16); I("gv", [128, NKT, 128], BF16)
        I("rope_cs", [2, 128, NL], F32); I("gnw", [128, 2], F32); I("PmT", [128, 128], BF16)
        I("nq", [128, NT], BF16); I("nk", [128, NT], BF16); I("nve", [128, NKT, 128], BF16); I("nvo", [128, 127, 128], BF16)
        I("nbias", [9, 128, 4, 64], F32)
        I("rq", [64, NT], BF16); I("rk", [64, NT], BF16); I("rkt", [128, NKT, 64], BF16); I("rvt", [128, NKT, 128], BF16)
        I("rg", [128, NT], BF16); I("rrope", [2, 64, NT], F32); I("rropet", [2, 128, NKT, 32], F32)
        I("Pm64T", [64, 64], BF16); I("rld", [128, 2], F32); I("rtab", [128, 4, 128], F32); I("rpcol", [128, 2], F32)
        I("rqd", [64, 2, 128], F32); I("rgnw", [128, 1], F32)
        I("hz", [64, 128, 12, 256], BF16); I("hzc", [64, 128, 12, 4], BF16); I("hw", [128, 64, 3, 4], F32); I("hb", [128, 64, 2], F32)
        I("fw1", [33, 64], F32); I("fw2", [64, 64], F32); I("fw3", [64, 2, 128], F32); I("fsc", [64, 4], F32); I("ndl", [128, 1], F32)
        I("feat", [33, 2 * NL], F32); I("featc", [33, 512], F32); I("ident", [128, 128], F32); I("J", [128, 128], BF16)
        O("yg", [128, NT], BF16); O("yn", [128, NT], BF16); O("yr", [128, NT], BF16)
        O("yh", [64, 128, 256], BF16); O("yhc", [64, 128, 4], BF16)
        D_["hf"] = P.dram("hf", [128, 2 * NL], BF16); D_["hfc"] = P.dram("hfc", [128, 512], BF16)
        S = {}
        with P.scope():
            S["ones_bf"] = P.sb([128, 128], BF16, "ones_bf"); P.memset("dve", S["ones_bf"][:, :], 1.0)
            S["epsb"] = P.sb([128, 1], F32, "epsb"); P.memset("dve", S["epsb"][:, :], EPS)
            S["ps1"] = Pool_(P, 2, [128, 512], F32, "ps", "ps1_")
            ret_mixer(P, S, nc, D_)
            na_mixer(P, S, nc, D_)
            gqa_mixer(P, S, nc, D_)
            hyena_mixer(P, S, nc, D_)
        P.flush(final_bufs=[D_["yg"], D_["yn"], D_["yr"], D_["yh"], D_["yhc"]])
    return nc
import ml_dtypes
BF = ml_dtypes.bfloat16
_PROG = {}


def _prog(name):
    if name not in _PROG:
        _PROG[name] = {"A": build_A, "B": build_B, "C": build_C}[name]()
    return _PROG[name]


def _run(name, maps):
    res = run_bass_kernel_spmd(_prog(name), maps, core_ids=list(range(8)))
    return res.results


def wlay(w):
    K, N = w.shape
    return np.ascontiguousarray(w.reshape(K // 128, 128, N // 128, 128).transpose(2, 1, 0, 3))


def vlay(v):
    return np.ascontiguousarray(v.reshape(-1, 128).T)


def tiles_tok(a, nt):
    return np.ascontiguousarray(a[:nt * 128].reshape(nt, 128, a.shape[1]).transpose(1, 0, 2))


def consts():
    f32 = np.float32
    C = {}
    t = np.arange(NL)
    inv = (f32(10000.0) ** (-np.arange(32, dtype=f32) / f32(32))).astype(f32)
    ang_r = (t // 64).astype(f32)[:, None] * inv[None, :]
    ang_c = (t % 64).astype(f32)[:, None] * inv[None, :]
    ang = np.concatenate([ang_r, ang_r, ang_c, ang_c], axis=1)
    C["rope_cs"] = np.ascontiguousarray(np.stack([np.cos(ang).T, np.sin(ang).T]).astype(f32))
    def pm(n):
        m = np.zeros((n, n), f32)
        for d in range(n):
            if d % 64 < 32:
                m[d + 32, d] = -1
            else:
                m[d - 32, d] = 1
        return m.astype(BF)
    C["PmT"] = pm(128); C["Pm64T"] = pm(64)
    inv2 = (f32(10000.0) ** (-np.linspace(0.0, 1.0, 32, dtype=f32))).astype(f32)
    a2 = np.arange(NL, dtype=f32)[:, None] * inv2[None, :]
    cosl, sinl = np.cos(a2).astype(f32), np.sin(a2).astype(f32)
    cosf = np.concatenate([cosl, np.ones((256, 32), f32)], 0); sinf = np.concatenate([sinl, np.zeros((256, 32), f32)], 0)
    C["rrope"] = np.ascontiguousarray(np.stack([np.concatenate([cosf, cosf], 1).T, np.concatenate([sinf, sinf], 1).T]))
    C["rropet"] = np.ascontiguousarray(np.stack([tiles_tok(cosf, NKT), tiles_tok(sinf, NKT)]))
    m = np.arange(128)[:, None]; c = np.arange(128)[None, :]
    C["rtab"] = np.ascontiguousarray(np.stack([np.maximum(c - m, 0), (c >= m), np.maximum(m - c, 0), (m >= c)], axis=1).astype(f32))
    p = np.arange(128)
    C["rpcol"] = np.stack([127 - p, p], 1).astype(f32)
    cc = np.arange(128)
    C["rqd"] = np.ascontiguousarray(np.broadcast_to(np.stack([cc + 1, 128 - cc])[None], (64, 2, 128)).astype(f32))
    C["ident"] = np.eye(128, dtype=f32)
    C["J"] = np.eye(128, dtype=f32)[::-1].astype(BF)
    def feat(L):
        x = np.arange(2 * L)
        s = np.abs(x - L).astype(f32)
        tn = s / f32(max(L - 1, 1))
        w = f32(2.0 * math.pi) * s / f32(L)
        f = np.linspace(1e-4, 15, 16, dtype=f32)
        return np.ascontiguousarray(np.concatenate([tn[None], np.cos(w[None] * f[:, None]), -np.sin(w[None] * f[:, None])], 0).astype(f32))
    C["feat"] = feat(NL); C["featc"] = feat(256)
    C["deltas"] = np.abs(np.linspace(math.log(1e-2) / 1.5, math.log(1e-2) / 0.3, 512, dtype=f32))
    return C


def na_bias(rpb_h):
    out = np.full((9, 128, 4, 64), -30000.0, np.float32)
    qc = np.arange(64)
    c0 = np.clip(qc - 8, 0, 48)
    for cls in range(9):
        r = cls if cls < 4 else (100 if cls == 4 else 247 + cls)
        r0 = min(max(r - 4, 0), 248)
        for i in range(4):
            for p in range(128):
                kk = i * 128 + p
                w, kc = kk // 64, kk % 64
                ok = (kc >= c0) & (kc < c0 + 16)
                ci = np.clip(kc - qc + 15, 0, 30)
                vals = rpb_h[r0 + w - r + 7, ci]
                out[cls, p, i, :] = np.where(ok, vals, -30000.0)
    return out


def make_mapB(inp, layer, k, PL, PC, PF, C):
    f32 = np.float32
    cw, cb = inp["hy_conv_w"][layer], inp["hy_conv_b"][layer]
    w3 = inp["hy_ffn_w3"][layer]
    b, h = k // 4, k % 4
    F = PF[b]
    m = {}
    m["gq"] = np.ascontiguousarray(F[4608 + 128 * h:4608 + 128 * h + 128])
    m["gk"] = np.ascontiguousarray(F[5120 + 128 * (h // 2):5120 + 128 * (h // 2) + 128])
    m["gv"] = tiles_tok(F[5376 + 128 * (h // 2):5376 + 128 * (h // 2) + 128].T, NKT)
    m["rope_cs"] = C["rope_cs"]; m["PmT"] = C["PmT"]
    m["gnw"] = np.ascontiguousarray(np.stack([inp["gqa_q_norm_w"][layer], inp["gqa_k_norm_w"][layer]], 1).astype(f32))
    m["nq"] = np.ascontiguousarray(F[1536 + 128 * h:1536 + 128 * h + 128])
    m["nk"] = np.ascontiguousarray(F[2048 + 128 * h:2048 + 128 * h + 128])
    V = F[2560 + 128 * h:2560 + 128 * h + 128].T
    m["nve"] = tiles_tok(V, NKT); m["nvo"] = tiles_tok(V[64:], 127)
    m["nbias"] = na_bias(inp["na_rpb"][layer][h])
    m["rq"] = np.ascontiguousarray(F[3072 + 64 * h:3072 + 64 * h + 64])
    m["rk"] = np.ascontiguousarray(F[3328 + 64 * h:3328 + 64 * h + 64])
    m["rkt"] = tiles_tok(F[3328 + 64 * h:3328 + 64 * h + 64].T, NKT)
    m["rvt"] = tiles_tok(F[3584 + 128 * h:3584 + 128 * h + 128].T, NKT)
    m["rg"] = np.ascontiguousarray(F[4096 + 128 * h:4096 + 128 * h + 128])
    m["rrope"] = C["rrope"]; m["rropet"] = C["rropet"]; m["Pm64T"] = C["Pm64T"]
    m["rld"] = np.ascontiguousarray(np.broadcast_to(inp["ret_log_decay"][layer][:, h][None, :], (128, 2)).astype(f32))
    m["rtab"] = C["rtab"]; m["rpcol"] = C["rpcol"]; m["rqd"] = C["rqd"]
    m["rgnw"] = np.ascontiguousarray(inp["ret_gn_w"][layer][128 * h:128 * h + 128].reshape(128, 1).astype(f32))
    def zlay(arr):
        L_ = arr.shape[1]
        z = np.zeros((2, 1), arr.dtype)
        prev = np.concatenate([z, arr[:, :-1]], 1); nxt = np.concatenate([arr[:, 1:], z], 1)
        return [np.ascontiguousarray(a_.reshape(2, L_ // 128, 128).transpose(2, 0, 1).reshape(128, -1)) for a_ in (prev, arr, nxt)]
    def hzbuild(srcs):
        out = []
        for c in range(64):
            row = 64 * k + c
            vs = zlay(np.stack([s_[row] for s_ in srcs]))
            x1s = zlay(np.stack([s_[512 + row] for s_ in srcs]))
            x2s = zlay(np.stack([s_[1024 + row] for s_ in srcs]))
            vf = [np.ascontiguousarray(a_[::-1]) for a_ in vs]
            out.append(np.stack(vs + vf + x1s + x2s, axis=1))
        return np.ascontiguousarray(np.stack(out))
    m["hz"] = hzbuild(PL); m["hzc"] = hzbuild(PC)
    ch = 64 * k + np.arange(64)
    hw = np.stack([np.stack([cw[0, s * 512 + ch], cw[1, s * 512 + ch], cw[2, s * 512 + ch], cb[s * 512 + ch]], -1) for s in range(3)], 1)
    m["hw"] = np.ascontiguousarray(np.broadcast_to(hw[None], (128, 64, 3, 4)).astype(f32))
    m["hb"] = np.ascontiguousarray(np.broadcast_to(inp["hy_bias"][layer][:, ch].T[None], (128, 64, 2)).astype(f32))
    m["fw1"] = inp["hy_ffn_w1"][layer]; m["fw2"] = inp["hy_ffn_w2"][layer]
    m["fw3"] = np.ascontiguousarray(np.stack([np.concatenate([w3[:, o * 1024 + dr * 512 + ch] for o in range(2)], 1) for dr in range(2)], 1))
    m["fsc"] = np.ascontiguousarray(np.stack([inp["hy_ffn_b1"][layer], inp["hy_sin_freq"][layer][0], inp["hy_ffn_b2"][layer], inp["hy_sin_freq"][layer][1]], 1).astype(f32))
    m["ndl"] = np.ascontiguousarray(-np.concatenate([C["deltas"][ch], C["deltas"][ch]]).reshape(128, 1).astype(f32))
    m["feat"] = C["feat"]; m["featc"] = C["featc"]; m["ident"] = C["ident"]; m["J"] = C["J"]
    return m

def kernel(**inp):
    inp = {k: np.asarray(v) for k, v in inp.items()}
    f32 = np.float32
    C = consts()
    x, ctx = inp["x"], inp["ctx"]
    XT = []
    for k in range(8):
        b, q = k // 4, k % 4
        xs = np.concatenate([x[b, q * 4096:(q + 1) * 4096], ctx[b, q * 64:(q + 1) * 64]], axis=0)
        XT.append(np.ascontiguousarray(xs.T.reshape(16, 128, NTOK)))
    cc = [np.ascontiguousarray(np.stack([inp["c"][b].reshape(16, 128).T, inp["c_ctx"].reshape(16, 128).T], axis=-1)) for b in range(2)]
    fo = None
    for layer in range(2):
        aw, ab = inp["ada_w"][layer], inp["ada_b"][layer]
        wA = {"adaw": wlay(aw[:, 0:2 * D]), "adab": vlay(ab[0:2 * D]), "n1w": vlay(inp["norm1_w"][layer]),
              "win": wlay(inp["w_in"][layer][:, :PCOLS])}
        rA = _run("A", [dict(wA, xT=XT[k], cc=cc[k // 4]) for k in range(8)])
        PL, PC = [], []
        for b in range(2):
            ps = [np.asarray(rA[4 * b + q]["pT"]).reshape(PCOLS, NTOK) for q in range(4)]
            PL.append(np.concatenate([p_[:, :4096] for p_ in ps], axis=1))
            PC.append(np.concatenate([p_[:, 4096:] for p_ in ps], axis=1))
        PF = [np.concatenate([PL[b], PC[b]], axis=1) for b in range(2)]
        mapsB = [make_mapB(inp, layer, k, PL, PC, PF, C) for k in range(8)]
        rB = _run("B", mapsB)
        Y = [np.zeros((2048, NL + 256), BF) for _ in range(2)]
        for k in range(8):
            b, h = k // 4, k % 4
            Y[b][512 + 128 * h:512 + 128 * h + 128] = np.asarray(rB[k]["yn"])
            Y[b][1024 + 128 * h:1024 + 128 * h + 128] = np.asarray(rB[k]["yr"])
            Y[b][1536 + 128 * h:1536 + 128 * h + 128] = np.asarray(rB[k]["yg"])
            yh = np.asarray(rB[k]["yh"]).reshape(64, 128, 2, 128)
            yhc = np.asarray(rB[k]["yhc"]).reshape(64, 128, 2, 2)
            for b2 in range(2):
                Y[b2][64 * k:64 * k + 64, :NL] = yh[:, :, b2, :].transpose(0, 2, 1).reshape(64, NL)
                Y[b2][64 * k:64 * k + 64, NL:] = yhc[:, :, b2, :].transpose(0, 2, 1).reshape(64, 256)
        wC = {"adaw": wlay(aw), "adab": vlay(ab), "n1w": vlay(inp["norm1_w"][layer]), "n2w": vlay(inp["norm2_w"][layer]),
              "fnw": vlay(inp["final_norm_w"]), "wg": wlay(inp["w_in"][layer][:, PCOLS:]),
              "wbr": np.concatenate([wlay(inp["w_branch"][layer][i]) for i in range(4)], 0),
              "wo": wlay(inp["w_out"][layer]), "w13": wlay(inp["ffn_w13"][layer]), "w2": wlay(inp["ffn_w2"][layer])}
        mapsC = []
        for k in range(8):
            b, q = k // 4, k % 4
            yt = np.concatenate([Y[b][:, q * 4096:(q + 1) * 4096], Y[b][:, NL + q * 64:NL + (q + 1) * 64]], axis=1)
            mapsC.append(dict(wC, xT=XT[k], yT=np.ascontiguousarray(yt.reshape(16, 128, NTOK)), cc=cc[k // 4]))
        rC = _run("C", mapsC)
        XT = [np.asarray(rC[k]["xo"]) for k in range(8)]
        fo = [np.asarray(rC[k]["fo"]) for k in range(8)]
    out = np.zeros((2, NL, D), np.float32)
    for k in range(8):
        b, q = k // 4, k % 4
        out[b, q * 4096:(q + 1) * 4096] = fo[k].reshape(D, NTOK)[:, :4096].T
    return out
```
